# Optimizing a Trainium2 kernel written in Bass

```python
import jax, jax.numpy as jnp
from jax import lax
import numpy as np

D_MODEL = 1024
BATCH = 8
SEQ = 4096
DEPTH = 4

N_MIXERS = 2
N_GLA_LAYERS = (DEPTH + 1) // 2
N_SWA_LAYERS = DEPTH // 2

GLA_HEADS = 4
GLA_DK = D_MODEL // 2
GLA_DV = D_MODEL
GLA_HK = GLA_DK // GLA_HEADS
GLA_HV = GLA_DV // GLA_HEADS
GLA_GATE_RANK = 16
GLA_GATE_NORMALIZER = 16.0
GLA_CHUNK = 64
GLA_IN = 2 * GLA_DK + 2 * GLA_DV + GLA_GATE_RANK

SWA_HEAD_DIM = 64
SWA_Q_HEADS = D_MODEL // SWA_HEAD_DIM
SWA_GROUP = 8
SWA_KV_HEADS = SWA_Q_HEADS // SWA_GROUP
SWA_WINDOW = 128
SWA_BLOCK = SWA_WINDOW
SWA_IN = (SWA_Q_HEADS + 2 * SWA_KV_HEADS) * SWA_HEAD_DIM

D_FF = 4 * D_MODEL
NORM_EPS = 1e-6

kernel_name = "hybrid_gla_swa_sink_sqrelu_sandwich"


def rms_norm(x, w):
    xf = x.astype(jnp.float32)
    y = xf * lax.rsqrt(jnp.mean(xf * xf, axis=-1, keepdims=True) + NORM_EPS)
    return (y * w.astype(jnp.float32)).astype(x.dtype)


def gla_mixer(h, w_in, w_gate_up, b_gate_up, g_norm, w_out):
    B, T, _ = h.shape
    nc = T // GLA_CHUNK
    proj = h @ w_in
    q, k, v, g, glr = jnp.split(
        proj, [GLA_DK, 2 * GLA_DK, 2 * GLA_DK + GLA_DV, 2 * GLA_DK + 2 * GLA_DV], axis=-1)
    log_a = jax.nn.log_sigmoid((glr @ w_gate_up + b_gate_up).astype(jnp.float32)) / GLA_GATE_NORMALIZER

    def chunks(t, d):
        return t.reshape(B, nc, GLA_CHUNK, GLA_HEADS, d).astype(jnp.float32)

    qc = chunks(q, GLA_HK) * (GLA_HK ** -0.5)
    kc = chunks(k, GLA_HK)
    vc = chunks(v, GLA_HV)
    bc = jnp.cumsum(chunks(log_a, GLA_HK), axis=2)
    b_last = bc[:, :, -1:]
    q_dec = qc * jnp.exp(bc)
    k_inv = kc * jnp.exp(-bc)
    k_end = kc * jnp.exp(b_last - bc)

    causal = jnp.tril(jnp.ones((GLA_CHUNK, GLA_CHUNK), dtype=bool))
    att = jnp.einsum('bncha,bnsha->bnhcs', q_dec, k_inv)
    att = jnp.where(causal, att, 0.0)
    o_intra = jnp.einsum('bnhcs,bnshv->bnchv', att, vc)

    dS = jnp.einsum('bncha,bnchv->bnhav', k_end, vc)
    decay = jnp.exp(b_last[:, :, 0])

    def step(S, inp):
        dS_n, dec_n = inp
        return S * dec_n[..., None] + dS_n, S

    S0 = jnp.zeros((B, GLA_HEADS, GLA_HK, GLA_HV), jnp.float32)
    _, S_prev = lax.scan(step, S0, (jnp.moveaxis(dS, 1, 0), jnp.moveaxis(decay, 1, 0)))
    S_prev = jnp.moveaxis(S_prev, 0, 1)
    o_inter = jnp.einsum('bncha,bnhav->bnchv', q_dec, S_prev)

    o = (o_intra + o_inter).reshape(B, T, GLA_HEADS, GLA_HV)
    o = rms_norm(o, g_norm).reshape(B, T, GLA_DV)
    o = o * jax.nn.silu(g.astype(jnp.float32))
    return o.astype(h.dtype) @ w_out


def swa_sink_mixer(h, w_in, b_in, sinks, w_out, b_out):
    B, T, _ = h.shape
    nb = T // SWA_BLOCK
    proj = h @ w_in + b_in
    q, k, v = jnp.split(
        proj, [SWA_Q_HEADS * SWA_HEAD_DIM, (SWA_Q_HEADS + SWA_KV_HEADS) * SWA_HEAD_DIM], axis=-1)
    q = q.reshape(B, nb, SWA_BLOCK, SWA_KV_HEADS, SWA_GROUP, SWA_HEAD_DIM)

    def banded(t):
        t = t.reshape(B, nb, SWA_BLOCK, SWA_KV_HEADS, SWA_HEAD_DIM)
        prev = jnp.pad(t[:, :-1], ((0, 0), (1, 0), (0, 0), (0, 0), (0, 0)))
        return jnp.concatenate([prev, t], axis=2)

    kb, vb = banded(k), banded(v)
    s = jnp.einsum('bnqkgd,bnskd->bnkgqs', q, kb).astype(jnp.float32) * (SWA_HEAD_DIM ** -0.5)
    qi = jnp.arange(SWA_BLOCK)[:, None]
    sj = jnp.arange(2 * SWA_BLOCK)[None, :]
    band = (sj > qi) & (sj <= qi + SWA_WINDOW)
    not_first = jnp.arange(nb)[:, None, None] > 0
    valid = band[None] & (not_first | (sj >= SWA_BLOCK)[None])
    s = jnp.where(valid[None, :, None, None], s, -jnp.inf)

    sink = sinks.astype(jnp.float32).reshape(SWA_KV_HEADS, SWA_GROUP)[None, None, :, :, None, None]
    mx = jnp.maximum(jnp.max(s, axis=-1, keepdims=True), sink)
    e = jnp.exp(s - mx)
    p = e / (jnp.sum(e, axis=-1, keepdims=True) + jnp.exp(sink - mx))
    o = jnp.einsum('bnkgqs,bnskd->bnqkgd', p.astype(vb.dtype), vb)
    return o.reshape(B, T, SWA_Q_HEADS * SWA_HEAD_DIM) @ w_out + b_out


def sq_relu_mlp(h, w_up, w_down):
    return jnp.square(jax.nn.relu(h @ w_up)) @ w_down


def setup_inputs(seed: int = 0) -> dict:
    key = jax.random.key(seed)
    ks = jax.random.split(key, 20)

    def dense(k, shape, fan_in):
        return jax.random.normal(k, shape, jnp.float32) * (fan_in ** -0.5)

    def gain(k, shape):
        return 1.0 + 0.05 * jax.random.normal(k, shape, jnp.float32)

    def bias(k, shape):
        return 0.02 * jax.random.normal(k, shape, jnp.float32)

    return {
        "x": jax.random.normal(ks[0], (BATCH, SEQ, D_MODEL), jnp.float32),
        "ln_mix_pre": gain(ks[1], (DEPTH, D_MODEL)),
        "ln_mix_post": gain(ks[2], (DEPTH, D_MODEL)),
        "ln_mlp_pre": gain(ks[3], (DEPTH, D_MODEL)),
        "ln_mlp_post": gain(ks[4], (DEPTH, D_MODEL)),
        "gla_w_in": dense(ks[5], (N_GLA_LAYERS, D_MODEL, GLA_IN), D_MODEL),
        "gla_w_gate_up": dense(ks[6], (N_GLA_LAYERS, GLA_GATE_RANK, GLA_DK), GLA_GATE_RANK),
        "gla_b_gate_up": bias(ks[7], (N_GLA_LAYERS, GLA_DK)),
        "gla_g_norm": gain(ks[8], (N_GLA_LAYERS, GLA_HV)),
        "gla_w_out": dense(ks[9], (N_GLA_LAYERS, GLA_DV, D_MODEL), GLA_DV),
        "swa_w_in": dense(ks[10], (N_SWA_LAYERS, D_MODEL, SWA_IN), D_MODEL),
        "swa_b_in": bias(ks[11], (N_SWA_LAYERS, SWA_IN)),
        "swa_sinks": 0.5 * jax.random.normal(ks[12], (N_SWA_LAYERS, SWA_Q_HEADS), jnp.float32),
        "swa_w_out": dense(ks[13], (N_SWA_LAYERS, SWA_Q_HEADS * SWA_HEAD_DIM, D_MODEL), SWA_Q_HEADS * SWA_HEAD_DIM),
        "swa_b_out": bias(ks[14], (N_SWA_LAYERS, D_MODEL)),
        "mlp_w_up": dense(ks[15], (DEPTH, D_MODEL, D_FF), D_MODEL),
        "mlp_w_down": dense(ks[16], (DEPTH, D_FF, D_MODEL), D_FF),
    }


def reference(x, ln_mix_pre, ln_mix_post, ln_mlp_pre, ln_mlp_post,
              gla_w_in, gla_w_gate_up, gla_b_gate_up, gla_g_norm, gla_w_out,
              swa_w_in, swa_b_in, swa_sinks, swa_w_out, swa_b_out,
              mlp_w_up, mlp_w_down):
    for i in range(DEPTH):
        j = i // N_MIXERS
        h = rms_norm(x, ln_mix_pre[i])
        if i % N_MIXERS == 0:
            m = gla_mixer(h, gla_w_in[j], gla_w_gate_up[j], gla_b_gate_up[j], gla_g_norm[j], gla_w_out[j])
        else:
            m = swa_sink_mixer(h, swa_w_in[j], swa_b_in[j], swa_sinks[j], swa_w_out[j], swa_b_out[j])
        x = x + rms_norm(m, ln_mix_post[i]).astype(x.dtype)
        h = rms_norm(x, ln_mlp_pre[i])
        f = sq_relu_mlp(h, mlp_w_up[i], mlp_w_down[i])
        x = x + rms_norm(f, ln_mlp_post[i]).astype(x.dtype)
    return x
```

```python
import numpy as np
import ml_dtypes
from contextlib import ExitStack

import concourse.bass as bass
import concourse.mybir as mybir
from concourse.bass_utils import run_bass_kernel_spmd

F32 = mybir.dt.float32
BF16 = mybir.dt.bfloat16
AF = mybir.ActivationFunctionType
ALU = mybir.AluOpType

P = 128
D = 1024
TT = 512
NCH = 8
DFF = 4096
SEQ = 4096
DEPTH = 4
EPS = 1e-6
GLA_IN = 3088
SWA_IN = 1280
SLAB_ELEMS = 8192
NSLAB = 3

PRM_LN = 0
PRM_GB = 128
PRM_GN = 136
PRM_SBI = 140
PRM_SBO = 160
PRM_KB = 176
PRM_N = 180

C_ID = 0
C_GMASK = 128
C_MCUR = 256
C_MPREV = 384
C_RMASK = 512
C_NMCUR = 1024
C_NMPREV = 1536
C_IND = 2048
C_GMASK4 = 2560
CW = 3072
ARENA = 18048
QSCALE = 128.0 ** -0.5
import os as _os
GLA_STAGE = int(_os.environ.get('GLA_STAGE', '9'))
GLA_SUB = _os.environ.get('GLA_SUB', 'z')


class Buf:
    __slots__ = ("name", "w", "r", "dsem", "dcnt")

    def __init__(self, name):
        self.name = name
        self.w = None
        self.r = {}
        self.dsem = None
        self.dcnt = 0


class Eng:
    def __init__(self, name, sem):
        self.name = name
        self.sem = sem
        self.cnt = 0
        self.seen = {}
        self.prog = []
        self.pending = None


class K:
    def __init__(self, ntiles, sublayers):
        self.ntiles = ntiles
        self.sublayers = sublayers
        self.nc = bass.Bass("TRN2", target_bir_lowering=False)
        self.es = ExitStack()
        self.nsem = 0
        self.pidx = 0

    def sem(self, name):
        self.nsem += 1
        return self.es.enter_context(self.nc.semaphore(name))

    def sb(self, name, shape, dt):
        return self.es.enter_context(self.nc.sbuf_tensor(name, list(shape), dt))

    def ps(self, name, shape, dt):
        return self.es.enter_context(self.nc.psum_tensor(name, list(shape), dt))

    def dram(self, name, shape, dt, kind):
        return self.nc.dram_tensor(name, list(shape), dt, kind=kind)

    def _waits(self, E, reads, writes):
        deps = {}

        def add(st):
            sem, val, eng = st
            if eng is E and E.name == "pe":
                return
            k = id(sem)
            if k not in deps or deps[k][1] < val:
                deps[k] = (sem, val)

        for b in reads:
            if b.w is not None:
                add(b.w)
        for b in writes:
            if b.w is not None and b.w[2] is not E:
                add(b.w)
            for st in b.r.values():
                if st[2] is not E:
                    add(st)
        for k, (sem, val) in deps.items():
            if E.seen.get(k, 0) < val:
                E.seen[k] = val
                E.prog.append(("wait", sem, val))

    def op(self, E, fn, reads=(), writes=(), inc=True):
        n0 = len(E.prog)
        self._waits(E, reads, writes)
        if len(E.prog) > n0 and E.pending is not None:
            i = E.pending
            E.prog[i] = ("op", E.prog[i][1], True)
            E.cnt += 1
            E.pending = None
        stamp = (E.sem, E.cnt + 1, E)
        E.prog.append(("op", fn, inc))
        if inc:
            E.cnt += 1
            E.pending = None
        else:
            E.pending = len(E.prog) - 1
        for b in reads:
            b.r[id(E.sem)] = stamp
        for b in writes:
            b.w = stamp
            b.r = {}

    def dma(self, Q, out_ap, in_ap, owner, reads=(), writes=()):
        self._waits(Q, reads, writes)
        if owner.dsem is None:
            owner.dsem = self.sem("d_" + owner.name)
        owner.dcnt += 16
        stamp = (owner.dsem, owner.dcnt, None)
        sem = owner.dsem
        Q.prog.append(("dma", out_ap, in_ap, sem))
        for b in reads:
            b.r[id(sem)] = stamp
        for b in writes:
            b.w = stamp
            b.r = {}

    def emit(self, E, eng):
        for it in E.prog:
            if it[0] == "wait":
                eng.wait_ge(it[1], it[2])
            elif it[0] == "op":
                ins = it[1](eng)
                if it[2]:
                    ins.then_inc(E.sem, 1)
            elif it[0] == "dma":
                eng.dma_start(out=it[1], in_=it[2]).then_inc(it[3], 16)

    def mm(self, out, lhsT, rhs, start, stop, reads, writes, inc, **kw):
        self.op(self.pe, lambda e: e.matmul(out, lhsT, rhs, start=start, stop=stop, **kw),
                reads, writes, inc)

    def act(self, out, in_, func, reads, writes, bias=None, scale=None):
        kw = {}
        if bias is not None:
            kw["bias"] = bias
        if scale is not None:
            kw["scale"] = scale
        self.op(self.ac, lambda e: e.activation(out=out, in_=in_, func=func, **kw), reads, writes)

    def barrier(self):
        engs = [self.pe, self.ac, self.dv, self.po]
        for E in engs:
            for Fe in engs:
                if Fe is E or Fe.cnt == 0:
                    continue
                k = id(Fe.sem)
                if E.seen.get(k, 0) < Fe.cnt:
                    E.seen[k] = Fe.cnt
                    E.prog.append(("wait", Fe.sem, Fe.cnt))
        self.aoff = 0

    def carve(self, words, dt=None, inner=None):
        a = self.arena[:, self.aoff:self.aoff + words]
        self.aoff += words
        assert self.aoff <= ARENA, self.aoff
        if dt is BF16:
            a = a.bitcast(BF16)
        if inner is not None:
            a = a.rearrange("p (a b) -> p a b", b=inner)
        return a

    def psbank(self):
        i = self.pidx % 8
        self.pidx += 1
        return self.pst[i], self.psb[i]

    def build(self):
        nc = self.nc
        nt = self.ntiles
        T = nt * TT
        self.pe = Eng("pe", self.sem("s_pe"))
        self.ac = Eng("act", self.sem("s_act"))
        self.dv = Eng("dve", self.sem("s_dve"))
        self.po = Eng("pool", self.sem("s_pool"))
        self.sp = Eng("sp", self.sem("s_sp"))

        dr = {}
        dr["xT"] = self.dram("xT", [D, T], F32, "ExternalInput")
        dr["yT"] = self.dram("yT", [D, T], F32, "ExternalOutput")
        dr["prm"] = self.dram("prm", [P, PRM_N], F32, "ExternalInput")
        dr["cst"] = self.dram("cst", [P, CW], F32, "ExternalInput")
        dr["snkL"] = self.dram("snkL", [4, 512], F32, "ExternalInput")
        dr["bvrow"] = self.dram("bvrow", [1, 256], F32, "ExternalInput")
        wshapes = {
            "gla_w_in": [2, D, GLA_IN], "gla_w_gate_up": [2, 16, 512], "gla_w_out": [2, D, D],
            "swa_w_in": [2, D, SWA_IN], "swa_w_out": [2, D, D],
            "mlp_w_up": [4, D, DFF], "mlp_w_down": [4, DFF, D],
        }
        wb = {}
        for k, shp in wshapes.items():
            dr[k] = self.dram(k, shp, F32, "ExternalInput")
            wb[k] = self.dram(k + "_bf", shp, BF16, "Internal")
        self.dr, self.wb = dr, wb

        self.xT = self.sb("xT_sb", [P, NCH, TT], F32)
        self.xTb = [Buf(f"xT{c}") for c in range(NCH)]
        self.hT = self.sb("hT_sb", [P, NCH, TT], BF16)
        self.hTb = [Buf(f"hT{c}") for c in range(NCH)]
        self.sq = self.sb("sq_sb", [P, NCH, TT], BF16)
        self.sqb = [Buf(f"sq{c}") for c in range(NCH)]
        self.yb_t = self.sb("y_sb", [P, NCH, TT], F32)
        self.ybb = [Buf(f"y{c}") for c in range(NCH)]
        self.rstd = self.sb("rstd_sb", [P, TT], F32)
        self.rstdb = Buf("rstd")
        self.slab = [self.sb(f"slab{i}", [P, SLAB_ELEMS], BF16) for i in range(NSLAB)]
        self.slabb = [Buf(f"slab{i}") for i in range(NSLAB)]
        self.slab_i = 0
        self.prm = self.sb("prm_sb", [P, PRM_N], F32)
        self.prmb = Buf("prm")
        self.arena = self.sb("arena_sb", [P, ARENA], F32)
        self.aoff = 0
        self.constb = Buf("const")
        self.ones = self.sb("ones_sb", [P, P], BF16)
        self.onesb = self.constb
        self.one1 = self.sb("one1_sb", [P, P], BF16)
        self.ones256 = self.sb("ones256_sb", [P, P], BF16)
        self.identb = self.sb("identb_sb", [P, P], BF16)
        self.identf = self.sb("identf_sb", [P, P], F32)
        self.gmask4 = self.sb("gmask4_sb", [P, TT], F32)
        self.rmask = self.sb("rmask_sb", [P, TT], F32)
        self.nmcur = self.sb("nmcur_sb", [P, TT], BF16)
        self.nmprev = self.sb("nmprev_sb", [P, TT], BF16)
        self.ind = self.sb("ind_sb", [4, TT], BF16)
        self.snkL = self.sb("snkL_sb", [4, 512], BF16)
        self.bvb = self.sb("bvb_sb", [1, 256], BF16)
        self.negb = self.sb("negb_sb", [P, 8], F32)
        self.eps = self.sb("eps_sb", [P, 1], F32)
        self.epsb = self.constb
        self.S = [self.sb(f"S{j}", [P, 4, 256], F32) for j in range(2)]
        self.Sb = [[Buf(f"S{j}_{h}") for h in range(4)] for j in range(2)]
        self.SA = [self.sb(f"SA{j}", [P, 4, 256], BF16) for j in range(2)]
        self.SAb = [[Buf(f"SA{j}_{h}") for h in range(4)] for j in range(2)]
        self.SB = self.sb("SB", [P, 4, 256], BF16)
        self.SBb = [Buf(f"SB_{h}") for h in range(4)]
        self.kd = [[self.sb(f"kd{j}_{kk}", [P, 640], BF16) for kk in range(2)] for j in range(2)]
        self.kdb = [[Buf(f"kd{j}_{kk}") for kk in range(2)] for j in range(2)]
        self.vts = [self.sb(f"vts{j}", [P, 5, P], BF16) for j in range(2)]
        self.vtsb = [Buf(f"vts{j}") for j in range(2)]
        self.wglr = [self.sb(f"wglr{j}", [P, NCH, 16], BF16) for j in range(2)]
        self.wglrb = [Buf(f"wglr{j}") for j in range(2)]
        self.wg = [self.sb(f"wg{j}", [16, 512], BF16) for j in range(2)]
        self.wgb = [Buf(f"wg{j}") for j in range(2)]
        self.p_i = 0
        self.pst = [self.ps(f"ps{i}", [P, TT], F32) for i in range(8)]
        self.psb = [Buf(f"ps{i}") for i in range(8)]

        print("SBUF bytes remaining per partition:", nc.sbuf_bytes_remaining)
        self.dma(self.sp, self.prm[:, :], dr["prm"].ap(), self.prmb, writes=[self.prmb])
        cst = self.carve(CW)
        cstb = Buf("cst")
        self.dma(self.sp, cst, dr["cst"].ap(), cstb, writes=[cstb])
        snkst = self.carve(512)
        snkb = Buf("snkst")
        self.dma(self.sp, snkst[0:4, :], dr["snkL"].ap(), snkb, writes=[snkb])
        bvst = self.carve(256)
        bvstb = Buf("bvst")
        self.dma(self.sp, bvst[0:1, :], dr["bvrow"].ap(), bvstb, writes=[bvstb])
        po = self.po
        self.op(po, lambda e: e.memset(self.ones[:, :], 1.0 / D), writes=[self.constb])
        self.op(po, lambda e: e.memset(self.one1[:, :], 1.0), writes=[self.constb])
        self.op(po, lambda e: e.memset(self.ones256[:, :], 1.0 / 256.0), writes=[self.constb])
        self.op(po, lambda e: e.memset(self.eps[:, :], EPS), writes=[self.constb])
        for j in range(2):
            self.op(po, lambda e, j=j: e.memset(self.S[j][:, :, :], 0.0), writes=self.Sb[j])
            self.op(po, lambda e, j=j: e.memset(self.SA[j][:, :, :], 0.0), writes=self.SAb[j])
        self.op(po, lambda e: e.tensor_copy(self.identb[:, :], cst[:, C_ID:C_ID + P]), [cstb], [self.constb])
        self.op(po, lambda e: e.tensor_copy(self.identf[:, :], cst[:, C_ID:C_ID + P]), [cstb], [self.constb])
        self.op(po, lambda e: e.tensor_copy(self.gmask4[:, :], cst[:, C_GMASK4:C_GMASK4 + TT]), [cstb], [self.constb])
        self.op(po, lambda e: e.tensor_copy(self.rmask[:, :], cst[:, C_RMASK:C_RMASK + TT]), [cstb], [self.constb])
        self.op(po, lambda e: e.tensor_copy(self.nmcur[:, :], cst[:, C_NMCUR:C_NMCUR + TT]), [cstb], [self.constb])
        self.op(po, lambda e: e.tensor_copy(self.nmprev[:, :], cst[:, C_NMPREV:C_NMPREV + TT]), [cstb], [self.constb])
        self.op(po, lambda e: e.tensor_copy(self.ind[0:4, :], cst[0:4, C_IND:C_IND + TT]), [cstb], [self.constb])
        self.op(po, lambda e: e.tensor_copy(self.bvb[0:1, :], bvst[0:1, :]), [bvstb], [self.constb])
        self.op(po, lambda e: e.tensor_scalar(self.negb[:, :], self.prm[:, PRM_GB:PRM_GB + 8], -1.0, None, ALU.mult),
                [self.prmb], [self.constb])
        self.act(self.snkL[0:4, :], snkst[0:4, :], AF.Exp, [snkb], [self.constb])

        self.convb = {}
        order = []
        for (kind, l) in self.sublayers:
            j = l // 2
            if kind == "gla":
                order += [("gla_w_in", j), ("gla_w_gate_up", j), ("gla_w_out", j)]
            elif kind == "swa":
                order += [("swa_w_in", j), ("swa_w_out", j)]
            elif kind == "mlp":
                order += [("mlp_w_up", l), ("mlp_w_down", l)]
        for (k, j) in order:
            if (k, j) in self.convb:
                continue
            b = Buf(f"cv_{k}_{j}")
            self.convb[(k, j)] = b
            R, C = wshapes[k][1], wshapes[k][2]
            rows_per = max(1, min(R, (1 << 19) // C))
            r0 = 0
            while r0 < R:
                r1 = min(R, r0 + rows_per)
                self.dma(self.po, wb[k].ap()[j, r0:r1, :], dr[k].ap()[j, r0:r1, :], b, writes=[])
                r0 = r1
            b.w = (b.dsem, b.dcnt, None)

        for j in range(2):
            if ("gla_w_in", j) in self.convb:
                src = wb["gla_w_in"].ap()[j].rearrange("(a p) f -> p a f", p=P)[:, :, 3072:3088]
                self.dma(self.sp, self.wglr[j][:, :, :], src, self.wglrb[j],
                         reads=[self.convb[("gla_w_in", j)]], writes=[self.wglrb[j]])
                self.dma(self.sp, self.wg[j][:, :], wb["gla_w_gate_up"].ap()[j], self.wgb[j],
                         reads=[self.convb[("gla_w_gate_up", j)]], writes=[self.wgb[j]])
        self.barrier()

        xTd = dr["xT"].ap().rearrange("(c p) t -> p c t", p=P)
        yTd = dr["yT"].ap().rearrange("(c p) t -> p c t", p=P)
        self.xin = Buf("xin")
        self.xout = Buf("xout")
        for t in range(nt):
            tsl = slice(t * TT, (t + 1) * TT)
            self.dma(self.sp, self.xT[:, :, :], xTd[:, :, tsl], self.xin, writes=self.xTb)
            for (kind, l) in self.sublayers:
                if kind == "mlp":
                    self.mlp(l)
                elif kind == "gla":
                    self.gla(l, t)
                elif kind == "swa":
                    self.swa(l, t)
            self.dma(self.sp, yTd[:, :, tsl], self.xT[:, :, :], self.xout, reads=self.xTb)
        self.sp.prog.append(("wait", self.xout.dsem, self.xout.dcnt))

        with nc.allow_low_precision("bf16 matmul operands, fp32 accumulation"):
            with nc.Block() as block:
                @block.tensor
                def _(e):
                    self.emit(self.pe, e)

                @block.scalar
                def _(e):
                    self.emit(self.ac, e)

                @block.vector
                def _(e):
                    self.emit(self.dv, e)

                @block.gpsimd
                def _(e):
                    self.emit(self.po, e)

                @block.sync
                def _(e):
                    self.emit(self.sp, e)
        self.es.close()
        return nc

    def gain_ap(self, kind, l, c):
        col = PRM_LN + kind * 32 + l * 8 + c
        return self.prm[:, col:col + 1]

    def slab_load(self, key, j, cols):
        i = self.slab_i % NSLAB
        self.slab_i += 1
        st, sbuf = self.slab[i], self.slabb[i]
        w = self.wb[key].ap()[j]
        c0, c1 = cols
        n = c1 - c0
        src = w.rearrange("(a p) f -> p a f", p=P)[:, :, c0:c1]
        na = src.shape[1]
        dst = st[:, 0:na * n].rearrange("p (a f) -> p a f", f=n)
        self.dma(self.sp, dst, src, sbuf, reads=[self.convb[(key, j)]], writes=[sbuf])
        return dst, sbuf

    def sumsq_rstd(self, srcb):
        pt, pb = self.psbank()
        for c in range(NCH):
            self.mm(pt[:, :], self.ones[:, :], self.sq[:, c, :], c == 0, c == NCH - 1,
                    [self.sqb[c], self.onesb], [pb], c == NCH - 1)
        self.act(self.rstd[:, :], pt[:, :], AF.Sqrt, [pb, self.epsb], [self.rstdb], bias=self.eps[:, 0:1])
        self.op(self.dv, lambda e: e.reciprocal(self.rstd[:, :], self.rstd[:, :]),
                [self.rstdb], [self.rstdb])

    def prenorm(self, kind, l):
        for c in range(NCH):
            self.act(self.sq[:, c, :], self.xT[:, c, :], AF.Square, [self.xTb[c]], [self.sqb[c]])
        self.sumsq_rstd(None)
        for c in range(NCH):
            g = self.gain_ap(kind, l, c)
            self.op(self.dv, lambda e, c=c, g=g: e.scalar_tensor_tensor(
                self.hT[:, c, :], self.xT[:, c, :], g, self.rstd[:, :], ALU.mult, ALU.mult),
                [self.xTb[c], self.rstdb, self.prmb], [self.hTb[c]])

    def postnorm_residual(self, kind, l):
        self.sumsq_rstd(None)
        for c in range(NCH):
            g = self.gain_ap(kind, l, c)
            self.op(self.dv, lambda e, c=c, g=g: e.scalar_tensor_tensor(
                self.yb_t[:, c, :], self.yb_t[:, c, :], g, self.rstd[:, :], ALU.mult, ALU.mult),
                [self.ybb[c], self.rstdb, self.prmb], [self.ybb[c]])
            self.op(self.po, lambda e, c=c: e.tensor_tensor(
                self.xT[:, c, :], self.xT[:, c, :], self.yb_t[:, c, :], ALU.add),
                [self.ybb[c], self.xTb[c]], [self.xTb[c]])

    def evac_y(self, pt, pb, c, bias=None):
        self.act(self.yb_t[:, c, :], pt[:, :], AF.Identity, [pb] + ([self.prmb] if bias is not None else []),
                 [self.ybb[c]], bias=bias)
        self.op(self.po, lambda e, c=c: e.tensor_tensor(
            self.sq[:, c, :], self.yb_t[:, c, :], self.yb_t[:, c, :], ALU.mult),
            [self.ybb[c]], [self.sqb[c]])

    def mlp(self, l):
        self.barrier()
        hid = self.carve(8192, BF16, TT)
        hidb = [Buf(f"hid{c}") for c in range(32)]
        relu = [self.carve(512) for _ in range(3)]
        relub = [Buf(f"relu{i}") for i in range(3)]
        self.prenorm(2, l)
        for s_ in range(4):
            w, wbuf = self.slab_load("mlp_w_up", l, (s_ * 1024, (s_ + 1) * 1024))
            for j in range(8):
                fch = s_ * 8 + j
                pt, pb = self.psbank()
                for kc in range(NCH):
                    self.mm(pt[:, :], w[:, kc, j * P:(j + 1) * P], self.hT[:, kc, :],
                            kc == 0, kc == NCH - 1, [wbuf, self.hTb[kc]], [pb], kc == NCH - 1)
                ri = fch % 3
                rt, rb = relu[ri], relub[ri]
                self.act(rt[:, :], pt[:, :], AF.Relu, [pb], [rb])
                self.op(self.dv, lambda e, fch=fch, rt=rt: e.tensor_tensor(
                    hid[:, fch, :], rt[:, :], rt[:, :], ALU.mult),
                    [rb], [hidb[fch]])
        for g in range(4):
            w, wbuf = self.slab_load("mlp_w_down", l, (g * 256, (g + 1) * 256))
            for c2 in range(2):
                c = 2 * g + c2
                pt, pb = self.psbank()
                for fc in range(32):
                    self.mm(pt[:, :], w[:, fc, c2 * P:(c2 + 1) * P], hid[:, fc, :],
                            fc == 0, fc == 31, [wbuf, hidb[fc]], [pb], fc == 31)
                self.evac_y(pt, pb, c)
        self.postnorm_residual(3, l)

    def proj_fm(self, w, wbuf, col0, nchunks, sink):
        for i in range(nchunks):
            pt, pb = self.psbank()
            for kc in range(NCH):
                self.mm(pt[:, :], w[:, kc, col0 + i * P:col0 + (i + 1) * P], self.hT[:, kc, :],
                        kc == 0, kc == NCH - 1, [wbuf, self.hTb[kc]], [pb], kc == NCH - 1)
            sink(i, pt, pb)

    def out_proj(self, key, j, mo, mob, kind, l, bias_col0=None):
        w, wbuf = self.slab_load(key, j, (0, 1024))
        for dc in range(8):
            pt, pb = self.psbank()
            for c in range(8):
                self.mm(pt[:, :], w[:, c, dc * P:(dc + 1) * P], mo[:, c, :], c == 0, c == 7,
                        [wbuf, mob[c]], [pb], c == 7)
            bias = None
            if bias_col0 is not None:
                bias = self.prm[:, bias_col0 + dc:bias_col0 + dc + 1]
            self.evac_y(pt, pb, dc, bias=bias)
        self.postnorm_residual(kind, l)

    def swa(self, l, t):
        j = l // 2
        self.barrier()
        qT = self.carve(2048, BF16, TT)
        qTb = [Buf(f"qT{c}") for c in range(8)]
        mo = self.carve(2048, BF16, TT)
        mob = [Buf(f"mo{c}") for c in range(8)]
        pT = [self.carve(256, BF16) for _ in range(8)]
        pTb = [Buf(f"pT{i}") for i in range(8)]
        rd = [self.carve(512) for _ in range(2)]
        rdb = [Buf(f"rd{i}") for i in range(2)]
        kd, kdb = self.kd[j], self.kdb[j]
        vt, vtb = self.vts[j], self.vtsb[j]
        self.prenorm(0, l)

        w, wbuf = self.slab_load("swa_w_in", j, (0, 1024))

        def qsink(c, pt, pb):
            col = PRM_SBI + j * 10 + c
            self.act(qT[:, c, :], pt[:, :], AF.Identity, [pb, self.prmb], [qTb[c]],
                     bias=self.prm[:, col:col + 1])
        self.proj_fm(w, wbuf, 0, 8, qsink)

        w, wbuf = self.slab_load("swa_w_in", j, (1024, 1280))
        for kk in range(2):
            pt, pb = self.psbank()
            for half in range(2):
                for kc in range(NCH):
                    self.mm(pt[half * 64:(half + 1) * 64, :], w[:, kc, kk * 64:(kk + 1) * 64],
                            self.hT[:, kc, :], kc == 0, kc == NCH - 1, [wbuf, self.hTb[kc]], [pb],
                            kc == NCH - 1 and half == 1)
            col = PRM_KB + j * 2 + kk
            self.act(kd[kk][:, 128:640], pt[:, :], AF.Identity, [pb, self.prmb], [kdb[kk]],
                     bias=self.prm[:, col:col + 1])
        pt, pb = self.psbank()
        for blk in range(4):
            for kc in range(NCH):
                self.mm(pt[:, blk * P:(blk + 1) * P], self.hT[:, kc, blk * P:(blk + 1) * P],
                        w[:, kc, 128:256], kc == 0, False, [wbuf, self.hTb[kc]], [pb], False)
            self.mm(pt[:, blk * P:(blk + 1) * P], self.one1[0:1, :], self.bvb[0:1, j * P:(j + 1) * P],
                    False, True, [self.constb], [pb], blk == 3)
        self.act(vt[:, 1:5, :], pt[:, :].rearrange("p (a b) -> p a b", b=P), AF.Identity, [pb], [vtb])

        for blk in range(4):
            gblk = t * 4 + blk
            tok = slice(blk * P, (blk + 1) * P)
            for kk in range(2):
                qbufs = qTb[kk * 4:(kk + 1) * 4]
                kbs = ([] if gblk == 0 else [0]) + [1]
                ptiles = {}
                for kb in kbs:
                    kcol = blk * P + kb * P
                    nm = self.nmprev if kb == 0 else self.nmcur
                    for par in range(2):
                        pt, pb = self.psbank()
                        rows = slice(par * 64, (par + 1) * 64)
                        self.mm(pt[:, :], kd[kk][rows, kcol:kcol + P], qT[rows, kk * 4:(kk + 1) * 4, tok],
                                True, False, [kdb[kk]] + qbufs, [pb], False)
                        self.mm(pt[:, :], self.identb[:, :], nm[:, :], False, True, [self.constb], [pb], True)
                        pi = self.p_i % 8
                        self.p_i += 1
                        self.act(pT[pi][:, :], pt[:, :], AF.Exp, [pb], [pTb[pi]], scale=0.125)
                        ptiles[(kb, par)] = (pT[pi], pTb[pi])
                po_t, po_b = self.psbank()
                pd_t, pd_b = self.psbank()
                for par in range(2):
                    rows = slice(par * 64, (par + 1) * 64)
                    for i, kb in enumerate(kbs):
                        ptile, pbuf = ptiles[(kb, par)]
                        self.mm(po_t[rows, :], vt[:, blk + kb, kk * 64:(kk + 1) * 64], ptile[:, :],
                                i == 0, i == len(kbs) - 1, [vtb, pbuf], [po_b], False)
                    for i, kb in enumerate(kbs):
                        ptile, pbuf = ptiles[(kb, par)]
                        self.mm(pd_t[rows, :], self.one1[:, 0:64], ptile[:, :], i == 0, False,
                                [pbuf, self.constb], [pd_b], False)
                    scol = ((j * 2 + kk) * 2 + par) * 64
                    self.mm(pd_t[rows, :], self.snkL[0:4, scol:scol + 64], self.ind[0:4, :], False, True,
                            [self.constb], [pd_b], par == 1)
                ri = (blk * 2 + kk) % 2
                self.op(self.dv, lambda e, ri=ri, pd_t=pd_t: e.reciprocal(rd[ri][:, :], pd_t[:, :]),
                        [pd_b], [rdb[ri]])
                self.op(self.dv, lambda e, ri=ri, po_t=po_t, kk=kk, tok=tok: e.tensor_tensor(
                    mo[:, kk * 4:(kk + 1) * 4, tok], po_t[:, :].rearrange("p (a b) -> p a b", b=P),
                    rd[ri][:, :].rearrange("p (a b) -> p a b", b=P), ALU.mult),
                    [po_b, rdb[ri]], mob[kk * 4:(kk + 1) * 4])
        for kk in range(2):
            self.op(self.po, lambda e, kk=kk: e.tensor_copy(kd[kk][:, 0:128], kd[kk][:, 512:640]),
                    [kdb[kk]], [kdb[kk]])
        self.op(self.po, lambda e: e.tensor_copy(vt[:, 0, :], vt[:, 4, :]), [vtb], [vtb])
        self.out_proj("swa_w_out", j, mo, mob, 1, l, bias_col0=PRM_SBO + j * 8)

    def gla(self, l, t):
        j = l // 2
        self.barrier()
        qd = self.carve(1024, BF16, TT)
        qdb = [Buf(f"qd{h}") for h in range(4)]
        kinv = self.carve(1024, BF16, TT)
        kinvb = [Buf(f"kinv{h}") for h in range(4)]
        kend = self.carve(2048, None, TT)
        kendb = [Buf(f"kend{h}") for h in range(4)]
        kendT = self.carve(1024, BF16, P)
        kendTb = [Buf(f"kendT{h}") for h in range(4)]
        vtok = self.carve(2048, BF16, 1024)
        vtokb = [Buf(f"vtok{b}") for b in range(4)]
        la = self.carve(512)
        lab = Buf("la")
        cpad = [self.carve(768, None, 96) for _ in range(2)]
        cpadb = [Buf(f"cpad{i}") for i in range(2)]
        for i in range(2):
            self.op(self.po, lambda e, i=i: e.memset(cpad[i][:, :, 0:32], 0.0), [], [cpadb[i]])
        E1 = [self.carve(512) for _ in range(2)]
        E1b = [Buf(f"E1_{i}") for i in range(2)]
        E2 = [self.carve(512) for _ in range(2)]
        E2b = [Buf(f"E2_{i}") for i in range(2)]
        oT = self.carve(4096, None, TT)
        oTb = [Buf(f"oT{c}") for c in range(8)]
        mo = self.carve(2048, BF16, TT)
        mob = [Buf(f"mo{c}") for c in range(8)]
        am = self.carve(256, BF16)
        amb = Buf("am")
        glrT = self.carve(256, BF16)
        glrTb = Buf("glrT")
        dec = self.carve(32)
        decb = Buf("dec")
        S, Sb = self.S[j], self.Sb[j]
        SA, SAb = self.SA[j], self.SAb[j]
        SB, SBb = self.SB, self.SBb
        self.prenorm(0, l)

        w, wbuf = self.slab_load("gla_w_in", j, (0, 1024))
        pt, pb = self.psbank()
        for kc in range(NCH):
            self.mm(pt[0:16, :], self.wglr[j][:, kc, :], self.hT[:, kc, :], kc == 0, kc == NCH - 1,
                    [self.wglrb[j], self.hTb[kc]], [pb], kc == NCH - 1)
        self.act(glrT[0:16, :], pt[0:16, :], AF.Identity, [pb], [glrTb])
        for h in range(4):
            if GLA_SUB <= 'a':
                continue
            pz, pzb = self.psbank()
            self.mm(pz[:, :], self.wg[j][0:16, h * P:(h + 1) * P], glrT[0:16, :], True, True,
                    [self.wgb[j], glrTb], [pzb], True)
            col = j * 4 + h
            self.act(la[:, :], pz[:, :], AF.Exp, [pzb, self.constb], [lab],
                     bias=self.negb[:, col:col + 1], scale=-1.0)
            self.act(la[:, :], la[:, :], AF.Ln, [lab], [lab], bias=1.0)
            if GLA_SUB <= 'b':
                continue
            src, srcb = cpad[0], cpadb[0]
            self.op(self.dv, lambda e, src=src: e.tensor_copy(src[:, :, 32:96], la[:, :].rearrange("p (a b) -> p a b", b=64)),
                    [lab], [srcb])
            k = 0
            for d in (1, 2, 4, 8, 16, 32):
                dst, dstb = cpad[1 - k], cpadb[1 - k]
                self.op(self.dv, lambda e, src=src, dst=dst, d=d: e.tensor_tensor(
                    dst[:, :, 32:96], src[:, :, 32:96], src[:, :, 32 - d:96 - d], ALU.add), [srcb], [dstb])
                src, srcb = dst, dstb
                k = 1 - k
            cumv = src[:, :, 32:96]
            cumb = srcb
            e1, e1b = E1[h % 2], E1b[h % 2]
            e2, e2b = E2[h % 2], E2b[h % 2]
            self.act(e1[:, :].rearrange("p (a b) -> p a b", b=64), cumv, AF.Exp, [cumb], [e1b], scale=-1.0 / 16.0)
            self.act(e2[:, :].rearrange("p (a b) -> p a b", b=64), cumv, AF.Exp, [cumb], [e2b], scale=1.0 / 16.0)
            self.act(dec[:, h * 8:(h + 1) * 8], src[:, :, 95], AF.Exp, [cumb], [decb], scale=-1.0 / 16.0)
            if GLA_SUB <= 'c':
                continue
            pq, pqb = self.psbank()
            for kc in range(NCH):
                self.mm(pq[:, :], w[:, kc, h * P:(h + 1) * P], self.hT[:, kc, :], kc == 0, kc == NCH - 1,
                        [wbuf, self.hTb[kc]], [pqb], kc == NCH - 1)
            self.op(self.dv, lambda e, h=h, pq=pq, e1=e1: e.scalar_tensor_tensor(
                qd[:, h, :], pq[:, :], QSCALE, e1[:, :], ALU.mult, ALU.mult), [pqb, e1b], [qdb[h]])
            pk, pkb = self.psbank()
            for kc in range(NCH):
                self.mm(pk[:, :], w[:, kc, 512 + h * P:512 + (h + 1) * P], self.hT[:, kc, :], kc == 0,
                        kc == NCH - 1, [wbuf, self.hTb[kc]], [pkb], kc == NCH - 1)
            self.op(self.dv, lambda e, h=h, pk=pk, e2=e2: e.tensor_tensor(
                kinv[:, h, :], pk[:, :], e2[:, :], ALU.mult), [pkb, e2b], [kinvb[h]])
            for n in range(8):
                cs = slice(n * 64, (n + 1) * 64)
                self.op(self.dv, lambda e, h=h, pk=pk, e2=e2, n=n, cs=cs: e.scalar_tensor_tensor(
                    kend[:, h, cs], pk[:, cs], dec[:, h * 8 + n:h * 8 + n + 1], e2[:, cs], ALU.mult, ALU.mult),
                    [pkb, e2b, decb], [kendb[h]])
            if GLA_SUB <= 'd':
                continue
            ptr, ptrb = self.psbank()
            for blk in range(4):
                self.op(self.pe, lambda e, h=h, blk=blk, ptr=ptr: e.transpose(
                    ptr[:, blk * P:(blk + 1) * P], kend[:, h, blk * P:(blk + 1) * P], self.identf[:, :]),
                    [kendb[h], self.constb], [ptrb], blk == 3)
            self.act(kendT[:, h * 4:(h + 1) * 4, :], ptr[:, :].rearrange("p (a b) -> p a b", b=P),
                     AF.Identity, [ptrb], [kendTb[h]])

        if GLA_STAGE <= 1:
            return
        w, wbuf = self.slab_load("gla_w_in", j, (1024, 2048))
        for blk in range(4):
            for half in range(2):
                pt, pb = self.psbank()
                for kc in range(NCH):
                    self.mm(pt[:, :], self.hT[:, kc, blk * P:(blk + 1) * P], w[:, kc, half * 512:(half + 1) * 512],
                            kc == 0, kc == NCH - 1, [wbuf, self.hTb[kc]], [pb], kc == NCH - 1)
                self.act(vtok[:, blk, half * 512:(half + 1) * 512], pt[:, :], AF.Identity, [pb], [vtokb[blk]])
        w, wbuf = self.slab_load("gla_w_in", j, (2048, 3072))

        def gsink(c, pt, pb):
            self.act(self.yb_t[:, c, :], pt[:, :], AF.Silu, [pb], [self.ybb[c]])
        self.proj_fm(w, wbuf, 0, 8, gsink)

        if GLA_STAGE <= 2:
            return
        for blk in range(4):
            tok = slice(blk * P, (blk + 1) * P)
            pa, pab = self.psbank()
            for h in range(4):
                self.mm(pa[:, h * P:(h + 1) * P], kinv[:, h, tok], qd[:, h, tok], h == 0, h == 3,
                        [kinvb[h], qdb[h]], [pab], h == 3, skip_group_check=True)
            self.op(self.dv, lambda e, pa=pa: e.tensor_tensor(am[:, :], pa[:, :], self.gmask4[:, :], ALU.mult),
                    [pab, self.constb], [amb])
            pdbank = [[self.psbank() for hp in range(2)] for ee in range(2)]
            for ee in range(2):
                rows = slice(ee * 64, (ee + 1) * 64)
                for h in range(4):
                    pdt, pdb = pdbank[ee][h // 2]
                    self.mm(pdt[:, (h % 2) * 256:(h % 2 + 1) * 256], kendT[rows, h * 4 + blk, :],
                            vtok[rows, blk, h * 256:(h + 1) * 256], h % 2 == 0, h % 2 == 1,
                            [kendTb[h], vtokb[blk]], [pdb], h % 2 == 1, skip_group_check=True)
            pos = [self.psbank() for _ in range(2)]
            for h in range(4):
                pot, pob = pos[h // 2]
                base = (h % 2) * 256
                pd0, pd0b = pdbank[0][h // 2]
                pd1, pd1b = pdbank[1][h // 2]
                hc = slice((h % 2) * 256, (h % 2 + 1) * 256)
                n0 = blk * 2
                for dvc in range(2):
                    self.mm(pot[:, base + dvc * P:base + (dvc + 1) * P],
                            vtok[:, blk, h * 256 + dvc * P:h * 256 + (dvc + 1) * P], am[:, h * P:(h + 1) * P],
                            (h % 2 == 0 and dvc == 0), False, [vtokb[blk], amb], [pob], False,
                            skip_group_check=True)
                for dvc in range(2):
                    self.mm(pot[:, base + dvc * P:base + dvc * P + 64], SA[:, h, dvc * P:(dvc + 1) * P],
                            qd[:, h, blk * P:blk * P + 64], False, False, [SAb[h], qdb[h]], [pob], dvc == 1,
                            skip_group_check=True)
                self.op(self.dv, lambda e, h=h, pd0=pd0, n0=n0, hc=hc: e.scalar_tensor_tensor(
                    S[:, h, :], S[:, h, :], dec[:, h * 8 + n0:h * 8 + n0 + 1], pd0[:, hc], ALU.mult, ALU.add),
                    [Sb[h], decb, pd0b], [Sb[h]])
                self.act(SB[:, h, :], S[:, h, :], AF.Identity, [Sb[h]], [SBb[h]])
                for dvc in range(2):
                    self.mm(pot[:, base + dvc * P + 64:base + (dvc + 1) * P], SB[:, h, dvc * P:(dvc + 1) * P],
                            qd[:, h, blk * P + 64:(blk + 1) * P], False, True, [SBb[h], qdb[h]], [pob], dvc == 1,
                            skip_group_check=True)
                self.op(self.dv, lambda e, h=h, pd1=pd1, n0=n0, hc=hc: e.scalar_tensor_tensor(
                    S[:, h, :], S[:, h, :], dec[:, h * 8 + n0 + 1:h * 8 + n0 + 2], pd1[:, hc], ALU.mult, ALU.add),
                    [Sb[h], decb, pd1b], [Sb[h]])
                self.act(SA[:, h, :], S[:, h, :], AF.Identity, [Sb[h]], [SAb[h]])
            for i in range(2):
                pot, pob = pos[i]
                self.act(oT[:, 4 * i:4 * i + 4, tok], pot[:, :].rearrange("p (a b) -> p a b", b=P), AF.Identity,
                         [pob], oTb[4 * i:4 * i + 4])
                self.op(self.po, lambda e, i=i, tok=tok: e.tensor_tensor(
                    self.sq[:, 4 * i:4 * i + 4, tok], oT[:, 4 * i:4 * i + 4, tok], oT[:, 4 * i:4 * i + 4, tok],
                    ALU.mult), oTb[4 * i:4 * i + 4], self.sqb[4 * i:4 * i + 4])

        if GLA_STAGE <= 3:
            return
        for h in range(4):
            pt, pb = self.psbank()
            for dvc in range(2):
                self.mm(pt[:, :], self.ones256[:, :], self.sq[:, 2 * h + dvc, :], dvc == 0, dvc == 1,
                        [self.sqb[2 * h + dvc], self.constb], [pb], dvc == 1)
            self.act(self.rstd[:, :], pt[:, :], AF.Sqrt, [pb, self.constb], [self.rstdb], bias=self.eps[:, 0:1])
            self.op(self.dv, lambda e: e.reciprocal(self.rstd[:, :], self.rstd[:, :]), [self.rstdb], [self.rstdb])
            for dvc in range(2):
                c = 2 * h + dvc
                gcol = PRM_GN + j * 2 + dvc
                self.op(self.dv, lambda e, c=c, gcol=gcol: e.scalar_tensor_tensor(
                    oT[:, c, :], oT[:, c, :], self.prm[:, gcol:gcol + 1], self.rstd[:, :], ALU.mult, ALU.mult),
                    [oTb[c], self.rstdb, self.prmb], [oTb[c]])
                self.op(self.po, lambda e, c=c: e.tensor_tensor(
                    mo[:, c, :], oT[:, c, :], self.yb_t[:, c, :], ALU.mult), [oTb[c], self.ybb[c]], [mob[c]])
        self.out_proj("gla_w_out", j, mo, mob, 1, l)


def _consts():
    c = np.zeros((P, CW), np.float32)
    c[:, C_ID:C_ID + P] = np.eye(P, dtype=np.float32)
    s = np.arange(P)[:, None]
    q = np.arange(P)[None, :]
    gm = ((s // 64 == q // 64) & (s <= q)).astype(np.float32)
    c[:, C_GMASK:C_GMASK + P] = gm
    c[:, C_GMASK4:C_GMASK4 + TT] = np.tile(gm, (1, 4))
    mcur = (s <= q)
    mprev = (s > q)
    c[:, C_MCUR:C_MCUR + P] = mcur.astype(np.float32)
    c[:, C_MPREV:C_MPREV + P] = mprev.astype(np.float32)
    c[:, C_NMCUR:C_NMCUR + TT] = np.tile(np.where(mcur, 0.0, -30000.0).astype(np.float32), (1, 4))
    c[:, C_NMPREV:C_NMPREV + TT] = np.tile(np.where(mprev, 0.0, -30000.0).astype(np.float32), (1, 4))
    tt = np.arange(TT)
    c[:, C_RMASK:C_RMASK + TT] = (tt % 64 != 0).astype(np.float32)[None, :]
    for cc in range(4):
        c[cc, C_IND + cc * P:C_IND + (cc + 1) * P] = 1.0
    return c


def _pack_params(inp):
    cols = []
    for k in ("ln_mix_pre", "ln_mix_post", "ln_mlp_pre", "ln_mlp_post"):
        cols.append(np.asarray(inp[k], np.float32).reshape(-1, P))
    cols.append(np.asarray(inp["gla_b_gate_up"], np.float32).reshape(-1, P))
    cols.append(np.asarray(inp["gla_g_norm"], np.float32).reshape(-1, P))
    bi = np.asarray(inp["swa_b_in"], np.float32)
    cols.append(bi.reshape(-1, P))
    cols.append(np.asarray(inp["swa_b_out"], np.float32).reshape(-1, P))
    for j in range(2):
        for kk in range(2):
            bk = bi[j, 1024 + kk * 64:1024 + (kk + 1) * 64]
            cols.append(np.concatenate([bk, bk])[None, :])
    prm = np.concatenate(cols, axis=0)
    assert prm.shape[0] == PRM_N, prm.shape
    return np.ascontiguousarray(prm.T)


def _pack_sinks(inp):
    s = np.asarray(inp["swa_sinks"], np.float32)
    out = np.zeros((4, 512), np.float32)
    for j in range(2):
        for kk in range(2):
            for par in range(2):
                col = ((j * 2 + kk) * 2 + par) * 64
                for cc in range(4):
                    out[cc, col:col + 64] = s[j, kk * 8 + 2 * cc + par]
    return out


FULL = [("gla", 0), ("mlp", 0), ("swa", 1), ("mlp", 1), ("gla", 2), ("mlp", 2), ("swa", 3), ("mlp", 3)]


def run(inp, sublayers=FULL, ntiles=SEQ // TT, ncores=8):
    x = np.asarray(inp["x"], np.float32)
    T = ntiles * TT
    kb = K(ntiles, sublayers)
    nc = kb.build()
    shared = {
        "prm": _pack_params(inp), "cst": _consts(), "snkL": _pack_sinks(inp),
        "bvrow": np.ascontiguousarray(np.asarray(inp["swa_b_in"], np.float32)[:, 1152:1280].reshape(1, 256)),
    }
    for k in ("gla_w_in", "gla_w_gate_up", "gla_w_out", "swa_w_in", "swa_w_out", "mlp_w_up", "mlp_w_down"):
        shared[k] = np.ascontiguousarray(np.asarray(inp[k], np.float32))
    in_maps = []
    for i in range(ncores):
        m = dict(shared)
        m["xT"] = np.ascontiguousarray(x[i, :T, :].T)
        in_maps.append(m)
    res = run_bass_kernel_spmd(nc, in_maps, core_ids=list(range(ncores)))
    out = np.stack([np.asarray(res.results[i]["yT"]).T for i in range(ncores)], axis=0)
    return np.ascontiguousarray(out.astype(np.float32))


def kernel(**inputs):
    return run(inputs)
```

```python
import numpy as np
import ml_dtypes
from contextlib import ExitStack

import concourse.bass as bass
import concourse.mybir as mybir
from concourse.bass_utils import run_bass_kernel_spmd

F32 = mybir.dt.float32
BF16 = mybir.dt.bfloat16
AF = mybir.ActivationFunctionType
ALU = mybir.AluOpType

P = 128
D = 1024
TT = 512
NCH = 8
DFF = 4096
SEQ = 4096
DEPTH = 4
EPS = 1e-6
GLA_IN = 3088
SWA_IN = 1280
SLAB_ELEMS = 8192
NSLAB = 3

PRM_LN = 0
PRM_GB = 128
PRM_GN = 136
PRM_SBI = 140
PRM_SBO = 160
PRM_KB = 176
PRM_N = 180

C_ID = 0
C_GMASK = 128
C_MCUR = 256
C_MPREV = 384
C_RMASK = 512
C_NMCUR = 1024
C_NMPREV = 1536
C_IND = 2048
C_GMASK4 = 2560
CW = 3072
ARENA = 18048
QSCALE = 128.0 ** -0.5
import os as _os
GLA_STAGE = int(_os.environ.get('GLA_STAGE', '9'))
GLA_SUB = _os.environ.get('GLA_SUB', 'z')


class Buf:
    __slots__ = ("name", "w", "r", "dsem", "dcnt")

    def __init__(self, name):
        self.name = name
        self.w = None
        self.r = {}
        self.dsem = None
        self.dcnt = 0


class Eng:
    def __init__(self, name, sem):
        self.name = name
        self.sem = sem
        self.cnt = 0
        self.seen = {}
        self.prog = []
        self.pending = None


class K:
    def __init__(self, ntiles, sublayers):
        self.ntiles = ntiles
        self.sublayers = sublayers
        self.nc = bass.Bass("TRN2", target_bir_lowering=False)
        self.es = ExitStack()
        self.nsem = 0
        self.pidx = 0

    def sem(self, name):
        self.nsem += 1
        return self.es.enter_context(self.nc.semaphore(name))

    def sb(self, name, shape, dt):
        return self.es.enter_context(self.nc.sbuf_tensor(name, list(shape), dt))

    def ps(self, name, shape, dt):
        return self.es.enter_context(self.nc.psum_tensor(name, list(shape), dt))

    def dram(self, name, shape, dt, kind):
        return self.nc.dram_tensor(name, list(shape), dt, kind=kind)

    def _waits(self, E, reads, writes):
        deps = {}

        def add(st):
            sem, val, eng = st
            if eng is E and E.name == "pe":
                return
            k = id(sem)
            if k not in deps or deps[k][1] < val:
                deps[k] = (sem, val)

        for b in reads:
            if b.w is not None:
                add(b.w)
        for b in writes:
            if b.w is not None and b.w[2] is not E:
                add(b.w)
            for st in b.r.values():
                if st[2] is not E:
                    add(st)
        for k, (sem, val) in deps.items():
            if E.seen.get(k, 0) < val:
                E.seen[k] = val
                E.prog.append(("wait", sem, val))

    def op(self, E, fn, reads=(), writes=(), inc=True):
        n0 = len(E.prog)
        self._waits(E, reads, writes)
        if len(E.prog) > n0 and E.pending is not None:
            i = E.pending
            E.prog[i] = ("op", E.prog[i][1], True)
            E.cnt += 1
            E.pending = None
        stamp = (E.sem, E.cnt + 1, E)
        E.prog.append(("op", fn, inc))
        if inc:
            E.cnt += 1
            E.pending = None
        else:
            E.pending = len(E.prog) - 1
        for b in reads:
            b.r[id(E.sem)] = stamp
        for b in writes:
            b.w = stamp
            b.r = {}

    def dma(self, Q, out_ap, in_ap, owner, reads=(), writes=()):
        self._waits(Q, reads, writes)
        if owner.dsem is None:
            owner.dsem = self.sem("d_" + owner.name)
        owner.dcnt += 16
        stamp = (owner.dsem, owner.dcnt, None)
        sem = owner.dsem
        Q.prog.append(("dma", out_ap, in_ap, sem))
        for b in reads:
            b.r[id(sem)] = stamp
        for b in writes:
            b.w = stamp
            b.r = {}

    def emit(self, E, eng):
        for it in E.prog:
            if it[0] == "wait":
                eng.wait_ge(it[1], it[2])
            elif it[0] == "op":
                ins = it[1](eng)
                if it[2]:
                    ins.then_inc(E.sem, 1)
            elif it[0] == "dma":
                eng.dma_start(out=it[1], in_=it[2]).then_inc(it[3], 16)

    def mm(self, out, lhsT, rhs, start, stop, reads, writes, inc, **kw):
        self.op(self.pe, lambda e: e.matmul(out, lhsT, rhs, start=start, stop=stop, **kw),
                reads, writes, inc)

    def act(self, out, in_, func, reads, writes, bias=None, scale=None):
        kw = {}
        if bias is not None:
            kw["bias"] = bias
        if scale is not None:
            kw["scale"] = scale
        self.op(self.ac, lambda e: e.activation(out=out, in_=in_, func=func, **kw), reads, writes)

    def barrier(self):
        engs = [self.pe, self.ac, self.dv, self.po]
        for E in engs[1:]:
            for Fe in engs:
                if Fe is E or Fe.cnt == 0:
                    continue
                k = id(Fe.sem)
                if E.seen.get(k, 0) < Fe.cnt:
                    E.seen[k] = Fe.cnt
                    E.prog.append(("wait", Fe.sem, Fe.cnt))
        self.aoff = 0

    def carve(self, words, dt=None, inner=None):
        a = self.arena[:, self.aoff:self.aoff + words]
        self.aoff += words
        assert self.aoff <= ARENA, self.aoff
        if dt is BF16:
            a = a.bitcast(BF16)
        if inner is not None:
            a = a.rearrange("p (a b) -> p a b", b=inner)
        return a

    def psbank(self):
        i = self.pidx % 8
        self.pidx += 1
        return self.pst[i], self.psb[i]

    def build(self):
        nc = self.nc
        nt = self.ntiles
        T = nt * TT
        self.pe = Eng("pe", self.sem("s_pe"))
        self.ac = Eng("act", self.sem("s_act"))
        self.dv = Eng("dve", self.sem("s_dve"))
        self.po = Eng("pool", self.sem("s_pool"))
        self.sp = Eng("sp", self.sem("s_sp"))

        dr = {}
        dr["xT"] = self.dram("xT", [D, T], F32, "ExternalInput")
        dr["yT"] = self.dram("yT", [D, T], F32, "ExternalOutput")
        dr["prm"] = self.dram("prm", [P, PRM_N], F32, "ExternalInput")
        dr["cst"] = self.dram("cst", [P, CW], F32, "ExternalInput")
        dr["snkL"] = self.dram("snkL", [4, 512], F32, "ExternalInput")
        dr["bvrow"] = self.dram("bvrow", [1, 256], F32, "ExternalInput")
        wshapes = {
            "gla_w_in": [2, D, GLA_IN], "gla_w_gate_up": [2, 16, 512], "gla_w_out": [2, D, D],
            "swa_w_in": [2, D, SWA_IN], "swa_w_out": [2, D, D],
            "mlp_w_up": [4, D, DFF], "mlp_w_down": [4, DFF, D],
        }
        wb = {}
        for k, shp in wshapes.items():
            dr[k] = self.dram(k, shp, F32, "ExternalInput")
            wb[k] = self.dram(k + "_bf", shp, BF16, "Internal")
        self.dr, self.wb = dr, wb

        self.xT = self.sb("xT_sb", [P, NCH, TT], F32)
        self.xTb = [Buf(f"xT{c}") for c in range(NCH)]
        self.hT = self.sb("hT_sb", [P, NCH, TT], BF16)
        self.hTb = [Buf(f"hT{c}") for c in range(NCH)]
        self.sq = self.sb("sq_sb", [P, NCH, TT], BF16)
        self.sqb = [Buf(f"sq{c}") for c in range(NCH)]
        self.yb_t = self.sb("y_sb", [P, NCH, TT], F32)
        self.ybb = [Buf(f"y{c}") for c in range(NCH)]
        self.rstd = self.sb("rstd_sb", [P, TT], F32)
        self.rstdb = Buf("rstd")
        self.slab = [self.sb(f"slab{i}", [P, SLAB_ELEMS], BF16) for i in range(NSLAB)]
        self.slabb = [Buf(f"slab{i}") for i in range(NSLAB)]
        self.slab_i = 0
        self.prm = self.sb("prm_sb", [P, PRM_N], F32)
        self.prmb = Buf("prm")
        self.arena = self.sb("arena_sb", [P, ARENA], F32)
        self.aoff = 0
        self.constb = Buf("const")
        self.ones = self.sb("ones_sb", [P, P], BF16)
        self.onesb = self.constb
        self.one1 = self.sb("one1_sb", [P, P], BF16)
        self.ones256 = self.sb("ones256_sb", [P, P], BF16)
        self.identb = self.sb("identb_sb", [P, P], BF16)
        self.identf = self.sb("identf_sb", [P, P], F32)
        self.gmask4 = self.sb("gmask4_sb", [P, TT], F32)
        self.rmask = self.sb("rmask_sb", [P, TT], F32)
        self.nmcur = self.sb("nmcur_sb", [P, TT], BF16)
        self.nmprev = self.sb("nmprev_sb", [P, TT], BF16)
        self.ind = self.sb("ind_sb", [4, TT], BF16)
        self.snkL = self.sb("snkL_sb", [4, 512], BF16)
        self.bvb = self.sb("bvb_sb", [1, 256], BF16)
        self.negb = self.sb("negb_sb", [P, 8], F32)
        self.eps = self.sb("eps_sb", [P, 1], F32)
        self.epsb = self.constb
        self.S = [self.sb(f"S{j}", [P, 4, 256], F32) for j in range(2)]
        self.Sb = [[Buf(f"S{j}_{h}") for h in range(4)] for j in range(2)]
        self.SA = [self.sb(f"SA{j}", [P, 4, 256], BF16) for j in range(2)]
        self.SAb = [[Buf(f"SA{j}_{h}") for h in range(4)] for j in range(2)]
        self.SB = self.sb("SB", [P, 4, 256], BF16)
        self.SBb = [Buf(f"SB_{h}") for h in range(4)]
        self.kd = [[self.sb(f"kd{j}_{kk}", [P, 640], BF16) for kk in range(2)] for j in range(2)]
        self.kdb = [[Buf(f"kd{j}_{kk}") for kk in range(2)] for j in range(2)]
        self.vts = [self.sb(f"vts{j}", [P, 5, P], BF16) for j in range(2)]
        self.vtsb = [Buf(f"vts{j}") for j in range(2)]
        self.wglr = [self.sb(f"wglr{j}", [P, NCH, 16], BF16) for j in range(2)]
        self.wglrb = [Buf(f"wglr{j}") for j in range(2)]
        self.wg = [self.sb(f"wg{j}", [16, 512], BF16) for j in range(2)]
        self.wgb = [Buf(f"wg{j}") for j in range(2)]
        self.p_i = 0
        self.pst = [self.ps(f"ps{i}", [P, TT], F32) for i in range(8)]
        self.psb = [Buf(f"ps{i}") for i in range(8)]

        self.dma(self.sp, self.prm[:, :], dr["prm"].ap(), self.prmb, writes=[self.prmb])
        cst = self.carve(CW)
        cstb = Buf("cst")
        self.dma(self.sp, cst, dr["cst"].ap(), cstb, writes=[cstb])
        snkst = self.carve(512)
        snkb = Buf("snkst")
        self.dma(self.sp, snkst[0:4, :], dr["snkL"].ap(), snkb, writes=[snkb])
        bvst = self.carve(256)
        bvstb = Buf("bvst")
        self.dma(self.sp, bvst[0:1, :], dr["bvrow"].ap(), bvstb, writes=[bvstb])
        po = self.po
        self.op(po, lambda e: e.memset(self.ones[:, :], 1.0 / D), writes=[self.constb])
        self.op(po, lambda e: e.memset(self.one1[:, :], 1.0), writes=[self.constb])
        self.op(po, lambda e: e.memset(self.ones256[:, :], 1.0 / 256.0), writes=[self.constb])
        self.op(po, lambda e: e.memset(self.eps[:, :], EPS), writes=[self.constb])
        for j in range(2):
            self.op(po, lambda e, j=j: e.memset(self.S[j][:, :, :], 0.0), writes=self.Sb[j])
            self.op(po, lambda e, j=j: e.memset(self.SA[j][:, :, :], 0.0), writes=self.SAb[j])
        self.op(po, lambda e: e.tensor_copy(self.identb[:, :], cst[:, C_ID:C_ID + P]), [cstb], [self.constb])
        self.op(po, lambda e: e.tensor_copy(self.identf[:, :], cst[:, C_ID:C_ID + P]), [cstb], [self.constb])
        self.op(po, lambda e: e.tensor_copy(self.gmask4[:, :], cst[:, C_GMASK4:C_GMASK4 + TT]), [cstb], [self.constb])
        self.op(po, lambda e: e.tensor_copy(self.rmask[:, :], cst[:, C_RMASK:C_RMASK + TT]), [cstb], [self.constb])
        self.op(po, lambda e: e.tensor_copy(self.nmcur[:, :], cst[:, C_NMCUR:C_NMCUR + TT]), [cstb], [self.constb])
        self.op(po, lambda e: e.tensor_copy(self.nmprev[:, :], cst[:, C_NMPREV:C_NMPREV + TT]), [cstb], [self.constb])
        self.op(po, lambda e: e.tensor_copy(self.ind[0:4, :], cst[0:4, C_IND:C_IND + TT]), [cstb], [self.constb])
        self.op(po, lambda e: e.tensor_copy(self.bvb[0:1, :], bvst[0:1, :]), [bvstb], [self.constb])
        self.op(po, lambda e: e.tensor_scalar(self.negb[:, :], self.prm[:, PRM_GB:PRM_GB + 8], -1.0, None, ALU.mult),
                [self.prmb], [self.constb])
        self.act(self.snkL[0:4, :], snkst[0:4, :], AF.Exp, [snkb], [self.constb])

        self.convb = {}
        order = []
        for (kind, l) in self.sublayers:
            j = l // 2
            if kind == "gla":
                order += [("gla_w_in", j), ("gla_w_gate_up", j), ("gla_w_out", j)]
            elif kind == "swa":
                order += [("swa_w_in", j), ("swa_w_out", j)]
            elif kind == "mlp":
                order += [("mlp_w_up", l), ("mlp_w_down", l)]
        for (k, j) in order:
            if (k, j) in self.convb:
                continue
            b = Buf(f"cv_{k}_{j}")
            self.convb[(k, j)] = b
            R, C = wshapes[k][1], wshapes[k][2]
            rows_per = max(1, min(R, (1 << 19) // C))
            r0 = 0
            while r0 < R:
                r1 = min(R, r0 + rows_per)
                self.dma(self.po, wb[k].ap()[j, r0:r1, :], dr[k].ap()[j, r0:r1, :], b, writes=[])
                r0 = r1
            b.w = (b.dsem, b.dcnt, None)

        for j in range(2):
            if ("gla_w_in", j) in self.convb:
                src = wb["gla_w_in"].ap()[j].rearrange("(a p) f -> p a f", p=P)[:, :, 3072:3088]
                self.dma(self.sp, self.wglr[j][:, :, :], src, self.wglrb[j],
                         reads=[self.convb[("gla_w_in", j)]], writes=[self.wglrb[j]])
                self.dma(self.sp, self.wg[j][:, :], wb["gla_w_gate_up"].ap()[j], self.wgb[j],
                         reads=[self.convb[("gla_w_gate_up", j)]], writes=[self.wgb[j]])
        self.barrier()

        xTd = dr["xT"].ap().rearrange("(c p) t -> p c t", p=P)
        yTd = dr["yT"].ap().rearrange("(c p) t -> p c t", p=P)
        self.xin = Buf("xin")
        self.xout = Buf("xout")
        for t in range(nt):
            tsl = slice(t * TT, (t + 1) * TT)
            self.dma(self.sp, self.xT[:, :, :], xTd[:, :, tsl], self.xin, writes=self.xTb)
            for (kind, l) in self.sublayers:
                if kind == "mlp":
                    self.mlp(l)
                elif kind == "gla":
                    self.gla(l, t)
                elif kind == "swa":
                    self.swa(l, t)
            self.dma(self.sp, yTd[:, :, tsl], self.xT[:, :, :], self.xout, reads=self.xTb)
        self.sp.prog.append(("wait", self.xout.dsem, self.xout.dcnt))

        with nc.allow_low_precision("bf16 matmul operands, fp32 accumulation"):
            with nc.Block() as block:
                @block.tensor
                def _(e):
                    self.emit(self.pe, e)

                @block.scalar
                def _(e):
                    self.emit(self.ac, e)

                @block.vector
                def _(e):
                    self.emit(self.dv, e)

                @block.gpsimd
                def _(e):
                    self.emit(self.po, e)

                @block.sync
                def _(e):
                    self.emit(self.sp, e)
        self.es.close()
        return nc

    def gain_ap(self, kind, l, c):
        col = PRM_LN + kind * 32 + l * 8 + c
        return self.prm[:, col:col + 1]

    def slab_load(self, key, j, cols):
        i = self.slab_i % NSLAB
        self.slab_i += 1
        st, sbuf = self.slab[i], self.slabb[i]
        w = self.wb[key].ap()[j]
        c0, c1 = cols
        n = c1 - c0
        src = w.rearrange("(a p) f -> p a f", p=P)[:, :, c0:c1]
        na = src.shape[1]
        dst = st[:, 0:na * n].rearrange("p (a f) -> p a f", f=n)
        self.dma(self.sp, dst, src, sbuf, reads=[self.convb[(key, j)]], writes=[sbuf])
        return dst, sbuf

    def sumsq_rstd(self, srcb):
        pt, pb = self.psbank()
        for c in range(NCH):
            self.mm(pt[:, :], self.ones[:, :], self.sq[:, c, :], c == 0, c == NCH - 1,
                    [self.sqb[c], self.onesb], [pb], c == NCH - 1)
        self.rstd_from(pt, pb)

    def rstd_from(self, pt, pb):
        self.act(self.rstd[:, :], pt[:, :], AF.Ln, [pb, self.constb], [self.rstdb], bias=self.eps[:, 0:1])
        self.act(self.rstd[:, :], self.rstd[:, :], AF.Exp, [self.rstdb], [self.rstdb], scale=-0.5)

    def prenorm(self, kind, l):
        for c in range(NCH):
            self.act(self.sq[:, c, :], self.xT[:, c, :], AF.Square, [self.xTb[c]], [self.sqb[c]])
        self.sumsq_rstd(None)
        for c in range(NCH):
            g = self.gain_ap(kind, l, c)
            self.op(self.dv, lambda e, c=c, g=g: e.scalar_tensor_tensor(
                self.hT[:, c, :], self.xT[:, c, :], g, self.rstd[:, :], ALU.mult, ALU.mult),
                [self.xTb[c], self.rstdb, self.prmb], [self.hTb[c]])

    def postnorm_residual(self, kind, l):
        self.sumsq_rstd(None)
        for c in range(NCH):
            g = self.gain_ap(kind, l, c)
            self.op(self.dv, lambda e, c=c, g=g: e.scalar_tensor_tensor(
                self.yb_t[:, c, :], self.yb_t[:, c, :], g, self.rstd[:, :], ALU.mult, ALU.mult),
                [self.ybb[c], self.rstdb, self.prmb], [self.ybb[c]])
            self.op(self.dv, lambda e, c=c: e.tensor_tensor(
                self.xT[:, c, :], self.xT[:, c, :], self.yb_t[:, c, :], ALU.add),
                [self.ybb[c], self.xTb[c]], [self.xTb[c]])

    def evac_y(self, pt, pb, c, bias=None):
        self.act(self.yb_t[:, c, :], pt[:, :], AF.Identity, [pb] + ([self.prmb] if bias is not None else []),
                 [self.ybb[c]], bias=bias)
        self.act(self.sq[:, c, :], pt[:, :], AF.Square, [pb] + ([self.prmb] if bias is not None else []),
                 [self.sqb[c]], bias=bias)

    def mlp(self, l):
        self.prenorm(2, l)
        self.barrier()
        hid = self.carve(8192, BF16, TT)
        hidb = [Buf(f"hid{c}") for c in range(32)]
        relu = [self.carve(512) for _ in range(3)]
        relub = [Buf(f"relu{i}") for i in range(3)]
        for s_ in range(4):
            w, wbuf = self.slab_load("mlp_w_up", l, (s_ * 1024, (s_ + 1) * 1024))
            for j in range(8):
                fch = s_ * 8 + j
                pt, pb = self.psbank()
                for kc in range(NCH):
                    self.mm(pt[:, :], w[:, kc, j * P:(j + 1) * P], self.hT[:, kc, :],
                            kc == 0, kc == NCH - 1, [wbuf, self.hTb[kc]], [pb], kc == NCH - 1)
                ri = fch % 3
                rt, rb = relu[ri], relub[ri]
                self.act(rt[:, :], pt[:, :], AF.Relu, [pb], [rb])
                self.op(self.dv, lambda e, fch=fch, rt=rt: e.tensor_tensor(
                    hid[:, fch, :], rt[:, :], rt[:, :], ALU.mult),
                    [rb], [hidb[fch]])
        for g in range(4):
            w, wbuf = self.slab_load("mlp_w_down", l, (g * 256, (g + 1) * 256))
            for c2 in range(2):
                c = 2 * g + c2
                pt, pb = self.psbank()
                for fc in range(32):
                    self.mm(pt[:, :], w[:, fc, c2 * P:(c2 + 1) * P], hid[:, fc, :],
                            fc == 0, fc == 31, [wbuf, hidb[fc]], [pb], fc == 31)
                self.evac_y(pt, pb, c)
        self.postnorm_residual(3, l)

    def proj_fm(self, w, wbuf, col0, nchunks, sink):
        for i in range(nchunks):
            pt, pb = self.psbank()
            for kc in range(NCH):
                self.mm(pt[:, :], w[:, kc, col0 + i * P:col0 + (i + 1) * P], self.hT[:, kc, :],
                        kc == 0, kc == NCH - 1, [wbuf, self.hTb[kc]], [pb], kc == NCH - 1)
            sink(i, pt, pb)

    def out_proj(self, key, j, mo, mob, kind, l, bias_col0=None):
        w, wbuf = self.slab_load(key, j, (0, 1024))
        for dc in range(8):
            pt, pb = self.psbank()
            for c in range(8):
                self.mm(pt[:, :], w[:, c, dc * P:(dc + 1) * P], mo[:, c, :], c == 0, c == 7,
                        [wbuf, mob[c]], [pb], c == 7)
            bias = None
            if bias_col0 is not None:
                bias = self.prm[:, bias_col0 + dc:bias_col0 + dc + 1]
            self.evac_y(pt, pb, dc, bias=bias)
        self.postnorm_residual(kind, l)

    def swa(self, l, t):
        j = l // 2
        self.prenorm(0, l)
        self.barrier()
        qT = self.carve(2048, BF16, TT)
        qTb = [Buf(f"qT{c}") for c in range(8)]
        mo = self.carve(2048, BF16, TT)
        mob = [Buf(f"mo{c}") for c in range(8)]
        pT = [self.carve(256, BF16) for _ in range(8)]
        pTb = [Buf(f"pT{i}") for i in range(8)]
        rd = [self.carve(512) for _ in range(2)]
        rdb = [Buf(f"rd{i}") for i in range(2)]
        kd, kdb = self.kd[j], self.kdb[j]
        vt, vtb = self.vts[j], self.vtsb[j]

        w, wbuf = self.slab_load("swa_w_in", j, (0, 1024))

        def qsink(c, pt, pb):
            col = PRM_SBI + j * 10 + c
            self.act(qT[:, c, :], pt[:, :], AF.Identity, [pb, self.prmb], [qTb[c]],
                     bias=self.prm[:, col:col + 1])
        self.proj_fm(w, wbuf, 0, 8, qsink)

        w, wbuf = self.slab_load("swa_w_in", j, (1024, 1280))
        for kk in range(2):
            pt, pb = self.psbank()
            for half in range(2):
                for kc in range(NCH):
                    self.mm(pt[half * 64:(half + 1) * 64, :], w[:, kc, kk * 64:(kk + 1) * 64],
                            self.hT[:, kc, :], kc == 0, kc == NCH - 1, [wbuf, self.hTb[kc]], [pb],
                            kc == NCH - 1 and half == 1)
            col = PRM_KB + j * 2 + kk
            self.act(kd[kk][:, 128:640], pt[:, :], AF.Identity, [pb, self.prmb], [kdb[kk]],
                     bias=self.prm[:, col:col + 1])
        pt, pb = self.psbank()
        for blk in range(4):
            for kc in range(NCH):
                self.mm(pt[:, blk * P:(blk + 1) * P], self.hT[:, kc, blk * P:(blk + 1) * P],
                        w[:, kc, 128:256], kc == 0, False, [wbuf, self.hTb[kc]], [pb], False)
            self.mm(pt[:, blk * P:(blk + 1) * P], self.one1[0:1, :], self.bvb[0:1, j * P:(j + 1) * P],
                    False, True, [self.constb], [pb], blk == 3)
        self.act(vt[:, 1:5, :], pt[:, :].rearrange("p (a b) -> p a b", b=P), AF.Identity, [pb], [vtb])

        for blk in range(4):
            gblk = t * 4 + blk
            tok = slice(blk * P, (blk + 1) * P)
            for kk in range(2):
                qbufs = qTb[kk * 4:(kk + 1) * 4]
                kbs = ([] if gblk == 0 else [0]) + [1]
                ptiles = {}
                for kb in kbs:
                    kcol = blk * P + kb * P
                    nm = self.nmprev if kb == 0 else self.nmcur
                    for par in range(2):
                        pt, pb = self.psbank()
                        rows = slice(par * 64, (par + 1) * 64)
                        self.mm(pt[:, :], kd[kk][rows, kcol:kcol + P], qT[rows, kk * 4:(kk + 1) * 4, tok],
                                True, False, [kdb[kk]] + qbufs, [pb], False)
                        self.mm(pt[:, :], self.identb[:, :], nm[:, :], False, True, [self.constb], [pb], True)
                        pi = self.p_i % 8
                        self.p_i += 1
                        self.act(pT[pi][:, :], pt[:, :], AF.Exp, [pb], [pTb[pi]], scale=0.125)
                        ptiles[(kb, par)] = (pT[pi], pTb[pi])
                po_t, po_b = self.psbank()
                pd_t, pd_b = self.psbank()
                for par in range(2):
                    rows = slice(par * 64, (par + 1) * 64)
                    for i, kb in enumerate(kbs):
                        ptile, pbuf = ptiles[(kb, par)]
                        self.mm(po_t[rows, :], vt[:, blk + kb, kk * 64:(kk + 1) * 64], ptile[:, :],
                                i == 0, i == len(kbs) - 1, [vtb, pbuf], [po_b], False)
                    for i, kb in enumerate(kbs):
                        ptile, pbuf = ptiles[(kb, par)]
                        self.mm(pd_t[rows, :], self.one1[:, 0:64], ptile[:, :], i == 0, False,
                                [pbuf, self.constb], [pd_b], False)
                    scol = ((j * 2 + kk) * 2 + par) * 64
                    self.mm(pd_t[rows, :], self.snkL[0:4, scol:scol + 64], self.ind[0:4, :], False, True,
                            [self.constb], [pd_b], par == 1)
                ri = (blk * 2 + kk) % 2
                self.act(rd[ri][:, :], pd_t[:, :], AF.Ln, [pd_b], [rdb[ri]])
                self.act(rd[ri][:, :], rd[ri][:, :], AF.Exp, [rdb[ri]], [rdb[ri]], scale=-1.0)
                self.op(self.dv, lambda e, ri=ri, po_t=po_t, kk=kk, tok=tok: e.tensor_tensor(
                    mo[:, kk * 4:(kk + 1) * 4, tok], po_t[:, :].rearrange("p (a b) -> p a b", b=P),
                    rd[ri][:, :].rearrange("p (a b) -> p a b", b=P), ALU.mult),
                    [po_b, rdb[ri]], mob[kk * 4:(kk + 1) * 4])
        for kk in range(2):
            self.op(self.po, lambda e, kk=kk: e.tensor_copy(kd[kk][:, 0:128], kd[kk][:, 512:640]),
                    [kdb[kk]], [kdb[kk]])
        self.op(self.po, lambda e: e.tensor_copy(vt[:, 0, :], vt[:, 4, :]), [vtb], [vtb])
        self.out_proj("swa_w_out", j, mo, mob, 1, l, bias_col0=PRM_SBO + j * 8)

    def gla(self, l, t):
        j = l // 2
        self.prenorm(0, l)
        self.barrier()
        qd = self.carve(1024, BF16, TT)
        qdb = [Buf(f"qd{h}") for h in range(4)]
        kinv = self.carve(1024, BF16, TT)
        kinvb = [Buf(f"kinv{h}") for h in range(4)]
        kend = self.carve(2048, None, TT)
        kendb = [Buf(f"kend{h}") for h in range(4)]
        kendT = self.carve(1024, BF16, P)
        kendTb = [Buf(f"kendT{h}") for h in range(4)]
        vtok = self.carve(2048, BF16, 1024)
        vtokb = [Buf(f"vtok{b}") for b in range(4)]
        la = self.carve(512)
        lab = Buf("la")
        cpad = [self.carve(768, None, 96) for _ in range(2)]
        cpadb = [Buf(f"cpad{i}") for i in range(2)]
        for i in range(2):
            self.op(self.po, lambda e, i=i: e.memset(cpad[i][:, :, 0:32], 0.0), [], [cpadb[i]])
        E1 = [self.carve(512) for _ in range(2)]
        E1b = [Buf(f"E1_{i}") for i in range(2)]
        E2 = [self.carve(512) for _ in range(2)]
        E2b = [Buf(f"E2_{i}") for i in range(2)]
        oT = self.carve(4096, None, TT)
        oTb = [Buf(f"oT{c}") for c in range(8)]
        mo = self.carve(2048, BF16, TT)
        mob = [Buf(f"mo{c}") for c in range(8)]
        am = self.carve(256, BF16)
        amb = Buf("am")
        glrT = self.carve(256, BF16)
        glrTb = Buf("glrT")
        dec = self.carve(32)
        decb = Buf("dec")
        S, Sb = self.S[j], self.Sb[j]
        SA, SAb = self.SA[j], self.SAb[j]
        SB, SBb = self.SB, self.SBb

        w, wbuf = self.slab_load("gla_w_in", j, (0, 1024))
        pt, pb = self.psbank()
        for kc in range(NCH):
            self.mm(pt[0:16, :], self.wglr[j][:, kc, :], self.hT[:, kc, :], kc == 0, kc == NCH - 1,
                    [self.wglrb[j], self.hTb[kc]], [pb], kc == NCH - 1)
        self.act(glrT[0:16, :], pt[0:16, :], AF.Identity, [pb], [glrTb])
        for h in range(4):
            if GLA_SUB <= 'a':
                continue
            pz, pzb = self.psbank()
            self.mm(pz[:, :], self.wg[j][0:16, h * P:(h + 1) * P], glrT[0:16, :], True, True,
                    [self.wgb[j], glrTb], [pzb], True)
            col = j * 4 + h
            self.act(la[:, :], pz[:, :], AF.Exp, [pzb, self.constb], [lab],
                     bias=self.negb[:, col:col + 1], scale=-1.0)
            self.act(la[:, :], la[:, :], AF.Ln, [lab], [lab], bias=1.0)
            if GLA_SUB <= 'b':
                continue
            src, srcb = cpad[0], cpadb[0]
            self.op(self.dv, lambda e, src=src: e.tensor_copy(src[:, :, 32:96], la[:, :].rearrange("p (a b) -> p a b", b=64)),
                    [lab], [srcb])
            k = 0
            for d in (1, 2, 4, 8, 16, 32):
                dst, dstb = cpad[1 - k], cpadb[1 - k]
                self.op(self.dv, lambda e, src=src, dst=dst, d=d: e.tensor_tensor(
                    dst[:, :, 32:96], src[:, :, 32:96], src[:, :, 32 - d:96 - d], ALU.add), [srcb], [dstb])
                src, srcb = dst, dstb
                k = 1 - k
            cumv = src[:, :, 32:96]
            cumb = srcb
            e1, e1b = E1[h % 2], E1b[h % 2]
            e2, e2b = E2[h % 2], E2b[h % 2]
            self.act(e1[:, :].rearrange("p (a b) -> p a b", b=64), cumv, AF.Exp, [cumb], [e1b], scale=-1.0 / 16.0)
            self.act(e2[:, :].rearrange("p (a b) -> p a b", b=64), cumv, AF.Exp, [cumb], [e2b], scale=1.0 / 16.0)
            self.act(dec[:, h * 8:(h + 1) * 8], src[:, :, 95], AF.Exp, [cumb], [decb], scale=-1.0 / 16.0)
            if GLA_SUB <= 'c':
                continue
            pq, pqb = self.psbank()
            for kc in range(NCH):
                self.mm(pq[:, :], w[:, kc, h * P:(h + 1) * P], self.hT[:, kc, :], kc == 0, kc == NCH - 1,
                        [wbuf, self.hTb[kc]], [pqb], kc == NCH - 1)
            self.op(self.dv, lambda e, h=h, pq=pq, e1=e1: e.scalar_tensor_tensor(
                qd[:, h, :], pq[:, :], QSCALE, e1[:, :], ALU.mult, ALU.mult), [pqb, e1b], [qdb[h]])
            pk, pkb = self.psbank()
            for kc in range(NCH):
                self.mm(pk[:, :], w[:, kc, 512 + h * P:512 + (h + 1) * P], self.hT[:, kc, :], kc == 0,
                        kc == NCH - 1, [wbuf, self.hTb[kc]], [pkb], kc == NCH - 1)
            self.op(self.dv, lambda e, h=h, pk=pk, e2=e2: e.tensor_tensor(
                kinv[:, h, :], pk[:, :], e2[:, :], ALU.mult), [pkb, e2b], [kinvb[h]])
            for n in range(8):
                cs = slice(n * 64, (n + 1) * 64)
                self.op(self.dv, lambda e, h=h, pk=pk, e2=e2, n=n, cs=cs: e.scalar_tensor_tensor(
                    kend[:, h, cs], pk[:, cs], dec[:, h * 8 + n:h * 8 + n + 1], e2[:, cs], ALU.mult, ALU.mult),
                    [pkb, e2b, decb], [kendb[h]])
            if GLA_SUB <= 'd':
                continue
            ptr, ptrb = self.psbank()
            for blk in range(4):
                self.op(self.pe, lambda e, h=h, blk=blk, ptr=ptr: e.transpose(
                    ptr[:, blk * P:(blk + 1) * P], kend[:, h, blk * P:(blk + 1) * P], self.identf[:, :]),
                    [kendb[h], self.constb], [ptrb], blk == 3)
            self.act(kendT[:, h * 4:(h + 1) * 4, :], ptr[:, :].rearrange("p (a b) -> p a b", b=P),
                     AF.Identity, [ptrb], [kendTb[h]])

        if GLA_STAGE <= 1:
            return
        w, wbuf = self.slab_load("gla_w_in", j, (1024, 2048))
        for blk in range(4):
            for half in range(2):
                pt, pb = self.psbank()
                for kc in range(NCH):
                    self.mm(pt[:, :], self.hT[:, kc, blk * P:(blk + 1) * P], w[:, kc, half * 512:(half + 1) * 512],
                            kc == 0, kc == NCH - 1, [wbuf, self.hTb[kc]], [pb], kc == NCH - 1)
                self.act(vtok[:, blk, half * 512:(half + 1) * 512], pt[:, :], AF.Identity, [pb], [vtokb[blk]])
        w, wbuf = self.slab_load("gla_w_in", j, (2048, 3072))

        def gsink(c, pt, pb):
            self.act(self.yb_t[:, c, :], pt[:, :], AF.Silu, [pb], [self.ybb[c]])
        self.proj_fm(w, wbuf, 0, 8, gsink)

        if GLA_STAGE <= 2:
            return
        for blk in range(4):
            tok = slice(blk * P, (blk + 1) * P)
            pa, pab = self.psbank()
            for h in range(4):
                self.mm(pa[:, h * P:(h + 1) * P], kinv[:, h, tok], qd[:, h, tok], h == 0, h == 3,
                        [kinvb[h], qdb[h]], [pab], h == 3, skip_group_check=True)
            self.op(self.dv, lambda e, pa=pa: e.tensor_tensor(am[:, :], pa[:, :], self.gmask4[:, :], ALU.mult),
                    [pab, self.constb], [amb])
            pdbank = [[self.psbank() for hp in range(2)] for ee in range(2)]
            for ee in range(2):
                rows = slice(ee * 64, (ee + 1) * 64)
                for h in range(4):
                    pdt, pdb = pdbank[ee][h // 2]
                    self.mm(pdt[:, (h % 2) * 256:(h % 2 + 1) * 256], kendT[rows, h * 4 + blk, :],
                            vtok[rows, blk, h * 256:(h + 1) * 256], h % 2 == 0, h % 2 == 1,
                            [kendTb[h], vtokb[blk]], [pdb], h % 2 == 1, skip_group_check=True)
            pos = [self.psbank() for _ in range(2)]
            for h in range(4):
                pot, pob = pos[h // 2]
                base = (h % 2) * 256
                pd0, pd0b = pdbank[0][h // 2]
                pd1, pd1b = pdbank[1][h // 2]
                hc = slice((h % 2) * 256, (h % 2 + 1) * 256)
                n0 = blk * 2
                for dvc in range(2):
                    self.mm(pot[:, base + dvc * P:base + (dvc + 1) * P],
                            vtok[:, blk, h * 256 + dvc * P:h * 256 + (dvc + 1) * P], am[:, h * P:(h + 1) * P],
                            (h % 2 == 0 and dvc == 0), False, [vtokb[blk], amb], [pob], False,
                            skip_group_check=True)
                for dvc in range(2):
                    self.mm(pot[:, base + dvc * P:base + dvc * P + 64], SA[:, h, dvc * P:(dvc + 1) * P],
                            qd[:, h, blk * P:blk * P + 64], False, False, [SAb[h], qdb[h]], [pob], dvc == 1,
                            skip_group_check=True)
                self.op(self.dv, lambda e, h=h, pd0=pd0, n0=n0, hc=hc: e.scalar_tensor_tensor(
                    S[:, h, :], S[:, h, :], dec[:, h * 8 + n0:h * 8 + n0 + 1], pd0[:, hc], ALU.mult, ALU.add),
                    [Sb[h], decb, pd0b], [Sb[h]])
                self.act(SB[:, h, :], S[:, h, :], AF.Identity, [Sb[h]], [SBb[h]])
                for dvc in range(2):
                    self.mm(pot[:, base + dvc * P + 64:base + (dvc + 1) * P], SB[:, h, dvc * P:(dvc + 1) * P],
                            qd[:, h, blk * P + 64:(blk + 1) * P], False, True, [SBb[h], qdb[h]], [pob], dvc == 1,
                            skip_group_check=True)
                self.op(self.dv, lambda e, h=h, pd1=pd1, n0=n0, hc=hc: e.scalar_tensor_tensor(
                    S[:, h, :], S[:, h, :], dec[:, h * 8 + n0 + 1:h * 8 + n0 + 2], pd1[:, hc], ALU.mult, ALU.add),
                    [Sb[h], decb, pd1b], [Sb[h]])
                self.act(SA[:, h, :], S[:, h, :], AF.Identity, [Sb[h]], [SAb[h]])
            for i in range(2):
                pot, pob = pos[i]
                self.act(oT[:, 4 * i:4 * i + 4, tok], pot[:, :].rearrange("p (a b) -> p a b", b=P), AF.Identity,
                         [pob], oTb[4 * i:4 * i + 4])
                self.op(self.po, lambda e, i=i, tok=tok: e.tensor_tensor(
                    self.sq[:, 4 * i:4 * i + 4, tok], oT[:, 4 * i:4 * i + 4, tok], oT[:, 4 * i:4 * i + 4, tok],
                    ALU.mult), oTb[4 * i:4 * i + 4], self.sqb[4 * i:4 * i + 4])

        if GLA_STAGE <= 3:
            return
        for h in range(4):
            pt, pb = self.psbank()
            for dvc in range(2):
                self.mm(pt[:, :], self.ones256[:, :], self.sq[:, 2 * h + dvc, :], dvc == 0, dvc == 1,
                        [self.sqb[2 * h + dvc], self.constb], [pb], dvc == 1)
            self.rstd_from(pt, pb)
            for dvc in range(2):
                c = 2 * h + dvc
                gcol = PRM_GN + j * 2 + dvc
                self.op(self.dv, lambda e, c=c, gcol=gcol: e.scalar_tensor_tensor(
                    oT[:, c, :], oT[:, c, :], self.prm[:, gcol:gcol + 1], self.rstd[:, :], ALU.mult, ALU.mult),
                    [oTb[c], self.rstdb, self.prmb], [oTb[c]])
                self.op(self.po, lambda e, c=c: e.tensor_tensor(
                    mo[:, c, :], oT[:, c, :], self.yb_t[:, c, :], ALU.mult), [oTb[c], self.ybb[c]], [mob[c]])
        self.out_proj("gla_w_out", j, mo, mob, 1, l)


def _consts():
    c = np.zeros((P, CW), np.float32)
    c[:, C_ID:C_ID + P] = np.eye(P, dtype=np.float32)
    s = np.arange(P)[:, None]
    q = np.arange(P)[None, :]
    gm = ((s // 64 == q // 64) & (s <= q)).astype(np.float32)
    c[:, C_GMASK:C_GMASK + P] = gm
    c[:, C_GMASK4:C_GMASK4 + TT] = np.tile(gm, (1, 4))
    mcur = (s <= q)
    mprev = (s > q)
    c[:, C_MCUR:C_MCUR + P] = mcur.astype(np.float32)
    c[:, C_MPREV:C_MPREV + P] = mprev.astype(np.float32)
    c[:, C_NMCUR:C_NMCUR + TT] = np.tile(np.where(mcur, 0.0, -30000.0).astype(np.float32), (1, 4))
    c[:, C_NMPREV:C_NMPREV + TT] = np.tile(np.where(mprev, 0.0, -30000.0).astype(np.float32), (1, 4))
    tt = np.arange(TT)
    c[:, C_RMASK:C_RMASK + TT] = (tt % 64 != 0).astype(np.float32)[None, :]
    for cc in range(4):
        c[cc, C_IND + cc * P:C_IND + (cc + 1) * P] = 1.0
    return c


def _pack_params(inp):
    cols = []
    for k in ("ln_mix_pre", "ln_mix_post", "ln_mlp_pre", "ln_mlp_post"):
        cols.append(np.asarray(inp[k], np.float32).reshape(-1, P))
    cols.append(np.asarray(inp["gla_b_gate_up"], np.float32).reshape(-1, P))
    cols.append(np.asarray(inp["gla_g_norm"], np.float32).reshape(-1, P))
    bi = np.asarray(inp["swa_b_in"], np.float32)
    cols.append(bi.reshape(-1, P))
    cols.append(np.asarray(inp["swa_b_out"], np.float32).reshape(-1, P))
    for j in range(2):
        for kk in range(2):
            bk = bi[j, 1024 + kk * 64:1024 + (kk + 1) * 64]
            cols.append(np.concatenate([bk, bk])[None, :])
    prm = np.concatenate(cols, axis=0)
    assert prm.shape[0] == PRM_N, prm.shape
    return np.ascontiguousarray(prm.T)


def _pack_sinks(inp):
    s = np.asarray(inp["swa_sinks"], np.float32)
    out = np.zeros((4, 512), np.float32)
    for j in range(2):
        for kk in range(2):
            for par in range(2):
                col = ((j * 2 + kk) * 2 + par) * 64
                for cc in range(4):
                    out[cc, col:col + 64] = s[j, kk * 8 + 2 * cc + par]
    return out


FULL = [("gla", 0), ("mlp", 0), ("swa", 1), ("mlp", 1), ("gla", 2), ("mlp", 2), ("swa", 3), ("mlp", 3)]


def run(inp, sublayers=FULL, ntiles=SEQ // TT, ncores=8):
    x = np.asarray(inp["x"], np.float32)
    T = ntiles * TT
    kb = K(ntiles, sublayers)
    nc = kb.build()
    shared = {
        "prm": _pack_params(inp), "cst": _consts(), "snkL": _pack_sinks(inp),
        "bvrow": np.ascontiguousarray(np.asarray(inp["swa_b_in"], np.float32)[:, 1152:1280].reshape(1, 256)),
    }
    for k in ("gla_w_in", "gla_w_gate_up", "gla_w_out", "swa_w_in", "swa_w_out", "mlp_w_up", "mlp_w_down"):
        shared[k] = np.ascontiguousarray(np.asarray(inp[k], np.float32))
    in_maps = []
    for i in range(ncores):
        m = dict(shared)
        m["xT"] = np.ascontiguousarray(x[i, :T, :].T)
        in_maps.append(m)
    res = run_bass_kernel_spmd(nc, in_maps, core_ids=list(range(ncores)))
    out = np.stack([np.asarray(res.results[i]["yT"]).T for i in range(ncores)], axis=0)
    return np.ascontiguousarray(out.astype(np.float32))


def kernel(**inputs):
    return run(inputs)
```

```python
import numpy as np
import ml_dtypes
from contextlib import ExitStack

import concourse.bass as bass
import concourse.mybir as mybir
from concourse.bass_utils import run_bass_kernel_spmd

F32 = mybir.dt.float32
BF16 = mybir.dt.bfloat16
AF = mybir.ActivationFunctionType
ALU = mybir.AluOpType

P = 128
D = 1024
TT = 512
NCH = 8
DFF = 4096
SEQ = 4096
DEPTH = 4
EPS = 1e-6
GLA_IN = 3088
SWA_IN = 1280
SLAB_ELEMS = 8192
NSLAB = 3

PRM_LN = 0
PRM_GB = 128
PRM_GN = 136
PRM_SBI = 140
PRM_SBO = 160
PRM_KB = 176
PRM_N = 180

C_ID = 0
C_GMASK = 128
C_MCUR = 256
C_MPREV = 384
C_RMASK = 512
C_NMCUR = 1024
C_NMPREV = 1536
C_IND = 2048
C_GMASK4 = 2560
CW = 3072
ARENA = 18048
QSCALE = 128.0 ** -0.5
import os as _os
GLA_STAGE = int(_os.environ.get('GLA_STAGE', '9'))
GLA_SUB = _os.environ.get('GLA_SUB', 'z')


class Buf:
    __slots__ = ("name", "w", "r", "dsem", "dcnt")

    def __init__(self, name):
        self.name = name
        self.w = None
        self.r = {}
        self.dsem = None
        self.dcnt = 0


class Eng:
    def __init__(self, name, sem):
        self.name = name
        self.sem = sem
        self.cnt = 0
        self.seen = {}
        self.prog = []
        self.pending = None


class K:
    def __init__(self, ntiles, sublayers):
        self.ntiles = ntiles
        self.sublayers = sublayers
        self.nc = bass.Bass("TRN2", target_bir_lowering=False)
        self.es = ExitStack()
        self.nsem = 0
        self.pidx = 0

    def sem(self, name):
        self.nsem += 1
        return self.es.enter_context(self.nc.semaphore(name))

    def sb(self, name, shape, dt):
        return self.es.enter_context(self.nc.sbuf_tensor(name, list(shape), dt))

    def ps(self, name, shape, dt):
        return self.es.enter_context(self.nc.psum_tensor(name, list(shape), dt))

    def dram(self, name, shape, dt, kind):
        return self.nc.dram_tensor(name, list(shape), dt, kind=kind)

    def _waits(self, E, reads, writes):
        deps = {}

        def add(st):
            sem, val, eng = st
            if eng is E and E.name == "pe":
                return
            k = id(sem)
            if k not in deps or deps[k][1] < val:
                deps[k] = (sem, val)

        for b in reads:
            if b.w is not None:
                add(b.w)
        for b in writes:
            if b.w is not None and b.w[2] is not E:
                add(b.w)
            for st in b.r.values():
                if st[2] is not E:
                    add(st)
        for k, (sem, val) in deps.items():
            if E.seen.get(k, 0) < val:
                E.seen[k] = val
                E.prog.append(("wait", sem, val))

    def op(self, E, fn, reads=(), writes=(), inc=True):
        n0 = len(E.prog)
        self._waits(E, reads, writes)
        if len(E.prog) > n0 and E.pending is not None:
            i = E.pending
            E.prog[i] = ("op", E.prog[i][1], True)
            E.cnt += 1
            E.pending = None
        stamp = (E.sem, E.cnt + 1, E)
        E.prog.append(("op", fn, inc))
        if inc:
            E.cnt += 1
            E.pending = None
        else:
            E.pending = len(E.prog) - 1
        for b in reads:
            b.r[id(E.sem)] = stamp
        for b in writes:
            b.w = stamp
            b.r = {}

    def dma(self, Q, out_ap, in_ap, owner, reads=(), writes=()):
        self._waits(Q, reads, writes)
        if owner.dsem is None:
            owner.dsem = self.sem("d_" + owner.name)
        owner.dcnt += 16
        stamp = (owner.dsem, owner.dcnt, None)
        sem = owner.dsem
        Q.prog.append(("dma", out_ap, in_ap, sem))
        for b in reads:
            b.r[id(sem)] = stamp
        for b in writes:
            b.w = stamp
            b.r = {}

    def emit(self, E, eng):
        for it in E.prog:
            if it[0] == "wait":
                eng.wait_ge(it[1], it[2])
            elif it[0] == "op":
                ins = it[1](eng)
                if it[2]:
                    ins.then_inc(E.sem, 1)
            elif it[0] == "dma":
                eng.dma_start(out=it[1], in_=it[2]).then_inc(it[3], 16)

    def mm(self, out, lhsT, rhs, start, stop, reads, writes, inc, **kw):
        self.op(self.pe, lambda e: e.matmul(out, lhsT, rhs, start=start, stop=stop, **kw),
                reads, writes, inc)

    def act(self, out, in_, func, reads, writes, bias=None, scale=None):
        kw = {}
        if bias is not None:
            kw["bias"] = bias
        if scale is not None:
            kw["scale"] = scale
        self.op(self.ac, lambda e: e.activation(out=out, in_=in_, func=func, **kw), reads, writes)

    def barrier(self):
        engs = [self.pe, self.ac, self.dv, self.po]
        for E in engs[1:]:
            for Fe in engs:
                if Fe is E or Fe.cnt == 0:
                    continue
                k = id(Fe.sem)
                if E.seen.get(k, 0) < Fe.cnt:
                    E.seen[k] = Fe.cnt
                    E.prog.append(("wait", Fe.sem, Fe.cnt))
        self.aoff = 0

    def carve(self, words, dt=None, inner=None):
        a = self.arena[:, self.aoff:self.aoff + words]
        self.aoff += words
        assert self.aoff <= ARENA, self.aoff
        if dt is BF16:
            a = a.bitcast(BF16)
        if inner is not None:
            a = a.rearrange("p (a b) -> p a b", b=inner)
        return a

    def psbank(self):
        i = self.pidx % 8
        self.pidx += 1
        return self.pst[i], self.psb[i]

    def build(self):
        nc = self.nc
        nt = self.ntiles
        T = nt * TT
        self.pe = Eng("pe", self.sem("s_pe"))
        self.ac = Eng("act", self.sem("s_act"))
        self.dv = Eng("dve", self.sem("s_dve"))
        self.po = Eng("pool", self.sem("s_pool"))
        self.sp = Eng("sp", self.sem("s_sp"))

        dr = {}
        dr["xT"] = self.dram("xT", [D, T], F32, "ExternalInput")
        dr["yT"] = self.dram("yT", [D, T], F32, "ExternalOutput")
        dr["prm"] = self.dram("prm", [P, PRM_N], F32, "ExternalInput")
        dr["cst"] = self.dram("cst", [P, CW], F32, "ExternalInput")
        dr["snkL"] = self.dram("snkL", [4, 512], F32, "ExternalInput")
        dr["bvrow"] = self.dram("bvrow", [1, 256], F32, "ExternalInput")
        wshapes = {
            "gla_w_in": [2, D, GLA_IN], "gla_w_gate_up": [2, 16, 512], "gla_w_out": [2, D, D],
            "swa_w_in": [2, D, SWA_IN], "swa_w_out": [2, D, D],
            "mlp_w_up": [4, D, DFF], "mlp_w_down": [4, DFF, D],
        }
        wb = {}
        for k, shp in wshapes.items():
            dr[k] = self.dram(k, shp, F32, "ExternalInput")
            wb[k] = self.dram(k + "_bf", shp, BF16, "Internal")
        self.dr, self.wb = dr, wb

        self.xT = self.sb("xT_sb", [P, NCH, TT], F32)
        self.xTb = [Buf(f"xT{c}") for c in range(NCH)]
        self.hT = self.sb("hT_sb", [P, NCH, TT], BF16)
        self.hTb = [Buf(f"hT{c}") for c in range(NCH)]
        self.sq = self.sb("sq_sb", [P, NCH, TT], BF16)
        self.sqb = [Buf(f"sq{c}") for c in range(NCH)]
        self.yb_t = self.sb("y_sb", [P, NCH, TT], F32)
        self.ybb = [Buf(f"y{c}") for c in range(NCH)]
        self.rstd = self.sb("rstd_sb", [P, TT], F32)
        self.rstdb = Buf("rstd")
        self.slab = [self.sb(f"slab{i}", [P, SLAB_ELEMS], BF16) for i in range(NSLAB)]
        self.slabb = [Buf(f"slab{i}") for i in range(NSLAB)]
        self.slab_i = 0
        self.prm = self.sb("prm_sb", [P, PRM_N], F32)
        self.prmb = Buf("prm")
        self.arena = self.sb("arena_sb", [P, ARENA], F32)
        self.aoff = 0
        self.constb = Buf("const")
        self.ones = self.sb("ones_sb", [P, P], BF16)
        self.onesb = self.constb
        self.one1 = self.sb("one1_sb", [P, P], BF16)
        self.ones256 = self.sb("ones256_sb", [P, P], BF16)
        self.identb = self.sb("identb_sb", [P, P], BF16)
        self.identf = self.sb("identf_sb", [P, P], F32)
        self.gmask4 = self.sb("gmask4_sb", [P, TT], F32)
        self.rmask = self.sb("rmask_sb", [P, TT], F32)
        self.nmcur = self.sb("nmcur_sb", [P, TT], BF16)
        self.nmprev = self.sb("nmprev_sb", [P, TT], BF16)
        self.ind = self.sb("ind_sb", [4, TT], BF16)
        self.snkL = self.sb("snkL_sb", [4, 512], BF16)
        self.bvb = self.sb("bvb_sb", [1, 256], BF16)
        self.negb = self.sb("negb_sb", [P, 8], F32)
        self.eps = self.sb("eps_sb", [P, 1], F32)
        self.epsb = self.constb
        self.S = [self.sb(f"S{j}", [P, 4, 256], F32) for j in range(2)]
        self.Sb = [[Buf(f"S{j}_{h}") for h in range(4)] for j in range(2)]
        self.SA = [self.sb(f"SA{j}", [P, 4, 256], BF16) for j in range(2)]
        self.SAb = [[Buf(f"SA{j}_{h}") for h in range(4)] for j in range(2)]
        self.SB = self.sb("SB", [P, 4, 256], BF16)
        self.SBb = [Buf(f"SB_{h}") for h in range(4)]
        self.kd = [[self.sb(f"kd{j}_{kk}", [P, 640], BF16) for kk in range(2)] for j in range(2)]
        self.kdb = [[Buf(f"kd{j}_{kk}") for kk in range(2)] for j in range(2)]
        self.vts = [self.sb(f"vts{j}", [P, 5, P], BF16) for j in range(2)]
        self.vtsb = [Buf(f"vts{j}") for j in range(2)]
        self.wglr = [self.sb(f"wglr{j}", [P, NCH, 16], BF16) for j in range(2)]
        self.wglrb = [Buf(f"wglr{j}") for j in range(2)]
        self.wg = [self.sb(f"wg{j}", [16, 512], BF16) for j in range(2)]
        self.wgb = [Buf(f"wg{j}") for j in range(2)]
        self.p_i = 0
        self.pst = [self.ps(f"ps{i}", [P, TT], F32) for i in range(8)]
        self.psb = [Buf(f"ps{i}") for i in range(8)]

        self.dma(self.sp, self.prm[:, :], dr["prm"].ap(), self.prmb, writes=[self.prmb])
        cst = self.carve(CW)
        cstb = Buf("cst")
        self.dma(self.sp, cst, dr["cst"].ap(), cstb, writes=[cstb])
        snkst = self.carve(512)
        snkb = Buf("snkst")
        self.dma(self.sp, snkst[0:4, :], dr["snkL"].ap(), snkb, writes=[snkb])
        bvst = self.carve(256)
        bvstb = Buf("bvst")
        self.dma(self.sp, bvst[0:1, :], dr["bvrow"].ap(), bvstb, writes=[bvstb])
        po = self.po
        self.op(po, lambda e: e.memset(self.ones[:, :], 1.0 / D), writes=[self.constb])
        self.op(po, lambda e: e.memset(self.one1[:, :], 1.0), writes=[self.constb])
        self.op(po, lambda e: e.memset(self.ones256[:, :], 1.0 / 256.0), writes=[self.constb])
        self.op(po, lambda e: e.memset(self.eps[:, :], EPS), writes=[self.constb])
        for j in range(2):
            self.op(po, lambda e, j=j: e.memset(self.S[j][:, :, :], 0.0), writes=self.Sb[j])
            self.op(po, lambda e, j=j: e.memset(self.SA[j][:, :, :], 0.0), writes=self.SAb[j])
        self.op(po, lambda e: e.tensor_copy(self.identb[:, :], cst[:, C_ID:C_ID + P]), [cstb], [self.constb])
        self.op(po, lambda e: e.tensor_copy(self.identf[:, :], cst[:, C_ID:C_ID + P]), [cstb], [self.constb])
        self.op(po, lambda e: e.tensor_copy(self.gmask4[:, :], cst[:, C_GMASK4:C_GMASK4 + TT]), [cstb], [self.constb])
        self.op(po, lambda e: e.tensor_copy(self.rmask[:, :], cst[:, C_RMASK:C_RMASK + TT]), [cstb], [self.constb])
        self.op(po, lambda e: e.tensor_copy(self.nmcur[:, :], cst[:, C_NMCUR:C_NMCUR + TT]), [cstb], [self.constb])
        self.op(po, lambda e: e.tensor_copy(self.nmprev[:, :], cst[:, C_NMPREV:C_NMPREV + TT]), [cstb], [self.constb])
        self.op(po, lambda e: e.tensor_copy(self.ind[0:4, :], cst[0:4, C_IND:C_IND + TT]), [cstb], [self.constb])
        self.op(po, lambda e: e.tensor_copy(self.bvb[0:1, :], bvst[0:1, :]), [bvstb], [self.constb])
        self.op(po, lambda e: e.tensor_scalar(self.negb[:, :], self.prm[:, PRM_GB:PRM_GB + 8], -1.0, None, ALU.mult),
                [self.prmb], [self.constb])
        self.act(self.snkL[0:4, :], snkst[0:4, :], AF.Exp, [snkb], [self.constb])

        self.convb = {}

        def conv(k, j, c0, c1):
            key = (k, j, c0, c1)
            if key in self.convb:
                return
            b = Buf(f"cv_{k}_{j}_{c0}")
            self.convb[key] = b
            R = wshapes[k][1]
            rows_per = max(1, min(R, (1 << 19) // (c1 - c0)))
            r0 = 0
            while r0 < R:
                r1 = min(R, r0 + rows_per)
                self.dma(self.po, wb[k].ap()[j, r0:r1, c0:c1], dr[k].ap()[j, r0:r1, c0:c1], b, writes=[])
                r0 = r1
            b.w = (b.dsem, b.dcnt, None)

        for (kind, l) in self.sublayers:
            j = l // 2
            if kind == "gla":
                conv("gla_w_in", j, 3072, 3088)
                conv("gla_w_gate_up", j, 0, 512)
                conv("gla_w_in", j, 0, 1024)
                conv("gla_w_in", j, 1024, 2048)
                conv("gla_w_in", j, 2048, 3072)
                conv("gla_w_out", j, 0, 1024)
            elif kind == "swa":
                conv("swa_w_in", j, 0, 1024)
                conv("swa_w_in", j, 1024, 1280)
                conv("swa_w_out", j, 0, 1024)
            elif kind == "mlp":
                for s_ in range(4):
                    conv("mlp_w_up", l, s_ * 1024, (s_ + 1) * 1024)
                for g in range(4):
                    conv("mlp_w_down", l, g * 256, (g + 1) * 256)
        self.small_loaded = set()
        self.barrier()

        xTd = dr["xT"].ap().rearrange("(c p) t -> p c t", p=P)
        yTd = dr["yT"].ap().rearrange("(c p) t -> p c t", p=P)
        self.xin = Buf("xin")
        self.xout = Buf("xout")
        for t in range(nt):
            tsl = slice(t * TT, (t + 1) * TT)
            self.dma(self.sp, self.xT[:, :, :], xTd[:, :, tsl], self.xin, writes=self.xTb)
            for (kind, l) in self.sublayers:
                if kind == "mlp":
                    self.mlp(l)
                elif kind == "gla":
                    self.gla(l, t)
                elif kind == "swa":
                    self.swa(l, t)
            self.dma(self.sp, yTd[:, :, tsl], self.xT[:, :, :], self.xout, reads=self.xTb)
        self.sp.prog.append(("wait", self.xout.dsem, self.xout.dcnt))

        with nc.allow_low_precision("bf16 matmul operands, fp32 accumulation"):
            with nc.Block() as block:
                @block.tensor
                def _(e):
                    self.emit(self.pe, e)

                @block.scalar
                def _(e):
                    self.emit(self.ac, e)

                @block.vector
                def _(e):
                    self.emit(self.dv, e)

                @block.gpsimd
                def _(e):
                    self.emit(self.po, e)

                @block.sync
                def _(e):
                    self.emit(self.sp, e)
        self.es.close()
        return nc

    def gain_ap(self, kind, l, c):
        col = PRM_LN + kind * 32 + l * 8 + c
        return self.prm[:, col:col + 1]

    def slab_load(self, key, j, cols):
        i = self.slab_i % NSLAB
        self.slab_i += 1
        st, sbuf = self.slab[i], self.slabb[i]
        w = self.wb[key].ap()[j]
        c0, c1 = cols
        n = c1 - c0
        src = w.rearrange("(a p) f -> p a f", p=P)[:, :, c0:c1]
        na = src.shape[1]
        dst = st[:, 0:na * n].rearrange("p (a f) -> p a f", f=n)
        self.dma(self.sp, dst, src, sbuf, reads=[self.convb[(key, j, c0, c1)]], writes=[sbuf])
        return dst, sbuf

    def load_small(self, j):
        if j in self.small_loaded:
            return
        self.small_loaded.add(j)
        src = self.wb["gla_w_in"].ap()[j].rearrange("(a p) f -> p a f", p=P)[:, :, 3072:3088]
        self.dma(self.sp, self.wglr[j][:, :, :], src, self.wglrb[j],
                 reads=[self.convb[("gla_w_in", j, 3072, 3088)]], writes=[self.wglrb[j]])
        self.dma(self.sp, self.wg[j][:, :], self.wb["gla_w_gate_up"].ap()[j], self.wgb[j],
                 reads=[self.convb[("gla_w_gate_up", j, 0, 512)]], writes=[self.wgb[j]])

    def sumsq_rstd(self, srcb):
        pt, pb = self.psbank()
        for c in range(NCH):
            self.mm(pt[:, :], self.ones[:, :], self.sq[:, c, :], c == 0, c == NCH - 1,
                    [self.sqb[c], self.onesb], [pb], c == NCH - 1)
        self.rstd_from(pt, pb)

    def rstd_from(self, pt, pb):
        self.act(self.rstd[:, :], pt[:, :], AF.Ln, [pb, self.constb], [self.rstdb], bias=self.eps[:, 0:1])
        self.act(self.rstd[:, :], self.rstd[:, :], AF.Exp, [self.rstdb], [self.rstdb], scale=-0.5)

    def prenorm(self, kind, l):
        for c in range(NCH):
            self.act(self.sq[:, c, :], self.xT[:, c, :], AF.Square, [self.xTb[c]], [self.sqb[c]])
        self.sumsq_rstd(None)
        for c in range(NCH):
            g = self.gain_ap(kind, l, c)
            self.op(self.dv, lambda e, c=c, g=g: e.scalar_tensor_tensor(
                self.hT[:, c, :], self.xT[:, c, :], g, self.rstd[:, :], ALU.mult, ALU.mult),
                [self.xTb[c], self.rstdb, self.prmb], [self.hTb[c]])

    def postnorm_residual(self, kind, l):
        self.sumsq_rstd(None)
        for c in range(NCH):
            g = self.gain_ap(kind, l, c)
            self.op(self.dv, lambda e, c=c, g=g: e.scalar_tensor_tensor(
                self.yb_t[:, c, :], self.yb_t[:, c, :], g, self.rstd[:, :], ALU.mult, ALU.mult),
                [self.ybb[c], self.rstdb, self.prmb], [self.ybb[c]])
            self.op(self.dv, lambda e, c=c: e.tensor_tensor(
                self.xT[:, c, :], self.xT[:, c, :], self.yb_t[:, c, :], ALU.add),
                [self.ybb[c], self.xTb[c]], [self.xTb[c]])

    def evac_y(self, pt, pb, c, bias=None):
        self.act(self.yb_t[:, c, :], pt[:, :], AF.Identity, [pb] + ([self.prmb] if bias is not None else []),
                 [self.ybb[c]], bias=bias)
        self.act(self.sq[:, c, :], pt[:, :], AF.Square, [pb] + ([self.prmb] if bias is not None else []),
                 [self.sqb[c]], bias=bias)

    def mlp(self, l):
        self.prenorm(2, l)
        self.barrier()
        hid = self.carve(8192, BF16, TT)
        hidb = [Buf(f"hid{c}") for c in range(32)]
        relu = [self.carve(512) for _ in range(3)]
        relub = [Buf(f"relu{i}") for i in range(3)]
        for s_ in range(4):
            w, wbuf = self.slab_load("mlp_w_up", l, (s_ * 1024, (s_ + 1) * 1024))
            for j in range(8):
                fch = s_ * 8 + j
                pt, pb = self.psbank()
                for kc in range(NCH):
                    self.mm(pt[:, :], w[:, kc, j * P:(j + 1) * P], self.hT[:, kc, :],
                            kc == 0, kc == NCH - 1, [wbuf, self.hTb[kc]], [pb], kc == NCH - 1)
                ri = fch % 3
                rt, rb = relu[ri], relub[ri]
                self.act(rt[:, :], pt[:, :], AF.Relu, [pb], [rb])
                self.op(self.dv, lambda e, fch=fch, rt=rt: e.tensor_tensor(
                    hid[:, fch, :], rt[:, :], rt[:, :], ALU.mult),
                    [rb], [hidb[fch]])
        for g in range(4):
            w, wbuf = self.slab_load("mlp_w_down", l, (g * 256, (g + 1) * 256))
            for c2 in range(2):
                c = 2 * g + c2
                pt, pb = self.psbank()
                for fc in range(32):
                    self.mm(pt[:, :], w[:, fc, c2 * P:(c2 + 1) * P], hid[:, fc, :],
                            fc == 0, fc == 31, [wbuf, hidb[fc]], [pb], fc == 31)
                self.evac_y(pt, pb, c)
        self.postnorm_residual(3, l)

    def proj_fm(self, w, wbuf, col0, nchunks, sink):
        for i in range(nchunks):
            pt, pb = self.psbank()
            for kc in range(NCH):
                self.mm(pt[:, :], w[:, kc, col0 + i * P:col0 + (i + 1) * P], self.hT[:, kc, :],
                        kc == 0, kc == NCH - 1, [wbuf, self.hTb[kc]], [pb], kc == NCH - 1)
            sink(i, pt, pb)

    def out_proj(self, key, j, mo, mob, kind, l, bias_col0=None):
        w, wbuf = self.slab_load(key, j, (0, 1024))
        for dc in range(8):
            pt, pb = self.psbank()
            for c in range(8):
                self.mm(pt[:, :], w[:, c, dc * P:(dc + 1) * P], mo[:, c, :], c == 0, c == 7,
                        [wbuf, mob[c]], [pb], c == 7)
            bias = None
            if bias_col0 is not None:
                bias = self.prm[:, bias_col0 + dc:bias_col0 + dc + 1]
            self.evac_y(pt, pb, dc, bias=bias)
        self.postnorm_residual(kind, l)

    def swa(self, l, t):
        j = l // 2
        self.prenorm(0, l)
        self.barrier()
        qT = self.carve(2048, BF16, TT)
        qTb = [Buf(f"qT{c}") for c in range(8)]
        mo = self.carve(2048, BF16, TT)
        mob = [Buf(f"mo{c}") for c in range(8)]
        pT = [self.carve(256, BF16) for _ in range(8)]
        pTb = [Buf(f"pT{i}") for i in range(8)]
        rd = [self.carve(512) for _ in range(2)]
        rdb = [Buf(f"rd{i}") for i in range(2)]
        kd, kdb = self.kd[j], self.kdb[j]
        vt, vtb = self.vts[j], self.vtsb[j]

        w, wbuf = self.slab_load("swa_w_in", j, (0, 1024))

        def qsink(c, pt, pb):
            col = PRM_SBI + j * 10 + c
            self.act(qT[:, c, :], pt[:, :], AF.Identity, [pb, self.prmb], [qTb[c]],
                     bias=self.prm[:, col:col + 1])
        self.proj_fm(w, wbuf, 0, 8, qsink)

        w, wbuf = self.slab_load("swa_w_in", j, (1024, 1280))
        for kk in range(2):
            pt, pb = self.psbank()
            for half in range(2):
                for kc in range(NCH):
                    self.mm(pt[half * 64:(half + 1) * 64, :], w[:, kc, kk * 64:(kk + 1) * 64],
                            self.hT[:, kc, :], kc == 0, kc == NCH - 1, [wbuf, self.hTb[kc]], [pb],
                            kc == NCH - 1 and half == 1)
            col = PRM_KB + j * 2 + kk
            self.act(kd[kk][:, 128:640], pt[:, :], AF.Identity, [pb, self.prmb], [kdb[kk]],
                     bias=self.prm[:, col:col + 1])
        pt, pb = self.psbank()
        for blk in range(4):
            for kc in range(NCH):
                self.mm(pt[:, blk * P:(blk + 1) * P], self.hT[:, kc, blk * P:(blk + 1) * P],
                        w[:, kc, 128:256], kc == 0, False, [wbuf, self.hTb[kc]], [pb], False)
            self.mm(pt[:, blk * P:(blk + 1) * P], self.one1[0:1, :], self.bvb[0:1, j * P:(j + 1) * P],
                    False, True, [self.constb], [pb], blk == 3)
        self.act(vt[:, 1:5, :], pt[:, :].rearrange("p (a b) -> p a b", b=P), AF.Identity, [pb], [vtb])

        for blk in range(4):
            gblk = t * 4 + blk
            tok = slice(blk * P, (blk + 1) * P)
            for kk in range(2):
                qbufs = qTb[kk * 4:(kk + 1) * 4]
                kbs = ([] if gblk == 0 else [0]) + [1]
                ptiles = {}
                for kb in kbs:
                    kcol = blk * P + kb * P
                    nm = self.nmprev if kb == 0 else self.nmcur
                    for par in range(2):
                        pt, pb = self.psbank()
                        rows = slice(par * 64, (par + 1) * 64)
                        self.mm(pt[:, :], kd[kk][rows, kcol:kcol + P], qT[rows, kk * 4:(kk + 1) * 4, tok],
                                True, False, [kdb[kk]] + qbufs, [pb], False)
                        self.mm(pt[:, :], self.identb[:, :], nm[:, :], False, True, [self.constb], [pb], True)
                        pi = self.p_i % 8
                        self.p_i += 1
                        self.act(pT[pi][:, :], pt[:, :], AF.Exp, [pb], [pTb[pi]], scale=0.125)
                        ptiles[(kb, par)] = (pT[pi], pTb[pi])
                po_t, po_b = self.psbank()
                pd_t, pd_b = self.psbank()
                for par in range(2):
                    rows = slice(par * 64, (par + 1) * 64)
                    for i, kb in enumerate(kbs):
                        ptile, pbuf = ptiles[(kb, par)]
                        self.mm(po_t[rows, :], vt[:, blk + kb, kk * 64:(kk + 1) * 64], ptile[:, :],
                                i == 0, i == len(kbs) - 1, [vtb, pbuf], [po_b], False)
                    for i, kb in enumerate(kbs):
                        ptile, pbuf = ptiles[(kb, par)]
                        self.mm(pd_t[rows, :], self.one1[:, 0:64], ptile[:, :], i == 0, False,
                                [pbuf, self.constb], [pd_b], False)
                    scol = ((j * 2 + kk) * 2 + par) * 64
                    self.mm(pd_t[rows, :], self.snkL[0:4, scol:scol + 64], self.ind[0:4, :], False, True,
                            [self.constb], [pd_b], par == 1)
                ri = (blk * 2 + kk) % 2
                self.act(rd[ri][:, :], pd_t[:, :], AF.Ln, [pd_b], [rdb[ri]])
                self.act(rd[ri][:, :], rd[ri][:, :], AF.Exp, [rdb[ri]], [rdb[ri]], scale=-1.0)
                self.op(self.dv, lambda e, ri=ri, po_t=po_t, kk=kk, tok=tok: e.tensor_tensor(
                    mo[:, kk * 4:(kk + 1) * 4, tok], po_t[:, :].rearrange("p (a b) -> p a b", b=P),
                    rd[ri][:, :].rearrange("p (a b) -> p a b", b=P), ALU.mult),
                    [po_b, rdb[ri]], mob[kk * 4:(kk + 1) * 4])
        for kk in range(2):
            self.op(self.dv, lambda e, kk=kk: e.tensor_copy(kd[kk][:, 0:128], kd[kk][:, 512:640]),
                    [kdb[kk]], [kdb[kk]])
        self.op(self.dv, lambda e: e.tensor_copy(vt[:, 0, :], vt[:, 4, :]), [vtb], [vtb])
        self.out_proj("swa_w_out", j, mo, mob, 1, l, bias_col0=PRM_SBO + j * 8)

    def gla(self, l, t):
        j = l // 2
        self.load_small(j)
        self.prenorm(0, l)
        self.barrier()
        qd = self.carve(1024, BF16, TT)
        qdb = [Buf(f"qd{h}") for h in range(4)]
        kinv = self.carve(1024, BF16, TT)
        kinvb = [Buf(f"kinv{h}") for h in range(4)]
        kend = self.carve(2048, None, TT)
        kendb = [Buf(f"kend{h}") for h in range(4)]
        kendT = self.carve(1024, BF16, P)
        kendTb = [Buf(f"kendT{h}") for h in range(4)]
        vtok = self.carve(2048, BF16, 1024)
        vtokb = [Buf(f"vtok{b}") for b in range(4)]
        la = self.carve(512)
        lab = Buf("la")
        cpad = [self.carve(768, None, 96) for _ in range(2)]
        cpadb = [Buf(f"cpad{i}") for i in range(2)]
        for i in range(2):
            self.op(self.dv, lambda e, i=i: e.memset(cpad[i][:, :, 0:32], 0.0), [], [cpadb[i]])
        E1 = [self.carve(512) for _ in range(2)]
        E1b = [Buf(f"E1_{i}") for i in range(2)]
        E2 = [self.carve(512) for _ in range(2)]
        E2b = [Buf(f"E2_{i}") for i in range(2)]
        oT = self.carve(4096, None, TT)
        oTb = [Buf(f"oT{c}") for c in range(8)]
        mo = self.carve(2048, BF16, TT)
        mob = [Buf(f"mo{c}") for c in range(8)]
        am = self.carve(256, BF16)
        amb = Buf("am")
        glrT = self.carve(256, BF16)
        glrTb = Buf("glrT")
        dec = self.carve(32)
        decb = Buf("dec")
        S, Sb = self.S[j], self.Sb[j]
        SA, SAb = self.SA[j], self.SAb[j]
        SB, SBb = self.SB, self.SBb

        w, wbuf = self.slab_load("gla_w_in", j, (0, 1024))
        pt, pb = self.psbank()
        for kc in range(NCH):
            self.mm(pt[0:16, :], self.wglr[j][:, kc, :], self.hT[:, kc, :], kc == 0, kc == NCH - 1,
                    [self.wglrb[j], self.hTb[kc]], [pb], kc == NCH - 1)
        self.act(glrT[0:16, :], pt[0:16, :], AF.Identity, [pb], [glrTb])
        for h in range(4):
            if GLA_SUB <= 'a':
                continue
            pz, pzb = self.psbank()
            self.mm(pz[:, :], self.wg[j][0:16, h * P:(h + 1) * P], glrT[0:16, :], True, True,
                    [self.wgb[j], glrTb], [pzb], True)
            col = j * 4 + h
            self.act(la[:, :], pz[:, :], AF.Exp, [pzb, self.constb], [lab],
                     bias=self.negb[:, col:col + 1], scale=-1.0)
            self.act(la[:, :], la[:, :], AF.Ln, [lab], [lab], bias=1.0)
            if GLA_SUB <= 'b':
                continue
            src, srcb = cpad[0], cpadb[0]
            self.op(self.dv, lambda e, src=src: e.tensor_copy(src[:, :, 32:96], la[:, :].rearrange("p (a b) -> p a b", b=64)),
                    [lab], [srcb])
            k = 0
            for d in (1, 2, 4, 8, 16, 32):
                dst, dstb = cpad[1 - k], cpadb[1 - k]
                self.op(self.dv, lambda e, src=src, dst=dst, d=d: e.tensor_tensor(
                    dst[:, :, 32:96], src[:, :, 32:96], src[:, :, 32 - d:96 - d], ALU.add), [srcb], [dstb])
                src, srcb = dst, dstb
                k = 1 - k
            cumv = src[:, :, 32:96]
            cumb = srcb
            e1, e1b = E1[h % 2], E1b[h % 2]
            e2, e2b = E2[h % 2], E2b[h % 2]
            self.act(e1[:, :].rearrange("p (a b) -> p a b", b=64), cumv, AF.Exp, [cumb], [e1b], scale=-1.0 / 16.0)
            self.act(e2[:, :].rearrange("p (a b) -> p a b", b=64), cumv, AF.Exp, [cumb], [e2b], scale=1.0 / 16.0)
            self.act(dec[:, h * 8:(h + 1) * 8], src[:, :, 95], AF.Exp, [cumb], [decb], scale=-1.0 / 16.0)
            if GLA_SUB <= 'c':
                continue
            pq, pqb = self.psbank()
            for kc in range(NCH):
                self.mm(pq[:, :], w[:, kc, h * P:(h + 1) * P], self.hT[:, kc, :], kc == 0, kc == NCH - 1,
                        [wbuf, self.hTb[kc]], [pqb], kc == NCH - 1)
            self.op(self.dv, lambda e, h=h, pq=pq, e1=e1: e.scalar_tensor_tensor(
                qd[:, h, :], pq[:, :], QSCALE, e1[:, :], ALU.mult, ALU.mult), [pqb, e1b], [qdb[h]])
            pk, pkb = self.psbank()
            for kc in range(NCH):
                self.mm(pk[:, :], w[:, kc, 512 + h * P:512 + (h + 1) * P], self.hT[:, kc, :], kc == 0,
                        kc == NCH - 1, [wbuf, self.hTb[kc]], [pkb], kc == NCH - 1)
            self.op(self.dv, lambda e, h=h, pk=pk, e2=e2: e.tensor_tensor(
                kinv[:, h, :], pk[:, :], e2[:, :], ALU.mult), [pkb, e2b], [kinvb[h]])
            for n in range(8):
                cs = slice(n * 64, (n + 1) * 64)
                self.op(self.dv, lambda e, h=h, pk=pk, e2=e2, n=n, cs=cs: e.scalar_tensor_tensor(
                    kend[:, h, cs], pk[:, cs], dec[:, h * 8 + n:h * 8 + n + 1], e2[:, cs], ALU.mult, ALU.mult),
                    [pkb, e2b, decb], [kendb[h]])
            if GLA_SUB <= 'd':
                continue
            ptr, ptrb = self.psbank()
            for blk in range(4):
                self.op(self.pe, lambda e, h=h, blk=blk, ptr=ptr: e.transpose(
                    ptr[:, blk * P:(blk + 1) * P], kend[:, h, blk * P:(blk + 1) * P], self.identf[:, :]),
                    [kendb[h], self.constb], [ptrb], blk == 3)
            self.act(kendT[:, h * 4:(h + 1) * 4, :], ptr[:, :].rearrange("p (a b) -> p a b", b=P),
                     AF.Identity, [ptrb], [kendTb[h]])

        if GLA_STAGE <= 1:
            return
        w, wbuf = self.slab_load("gla_w_in", j, (1024, 2048))
        for blk in range(4):
            for half in range(2):
                pt, pb = self.psbank()
                for kc in range(NCH):
                    self.mm(pt[:, :], self.hT[:, kc, blk * P:(blk + 1) * P], w[:, kc, half * 512:(half + 1) * 512],
                            kc == 0, kc == NCH - 1, [wbuf, self.hTb[kc]], [pb], kc == NCH - 1)
                self.act(vtok[:, blk, half * 512:(half + 1) * 512], pt[:, :], AF.Identity, [pb], [vtokb[blk]])
        w, wbuf = self.slab_load("gla_w_in", j, (2048, 3072))

        def gsink(c, pt, pb):
            self.act(self.yb_t[:, c, :], pt[:, :], AF.Silu, [pb], [self.ybb[c]])
        self.proj_fm(w, wbuf, 0, 8, gsink)

        if GLA_STAGE <= 2:
            return
        for blk in range(4):
            tok = slice(blk * P, (blk + 1) * P)
            pa, pab = self.psbank()
            for h in range(4):
                self.mm(pa[:, h * P:(h + 1) * P], kinv[:, h, tok], qd[:, h, tok], h == 0, h == 3,
                        [kinvb[h], qdb[h]], [pab], h == 3, skip_group_check=True)
            self.op(self.dv, lambda e, pa=pa: e.tensor_tensor(am[:, :], pa[:, :], self.gmask4[:, :], ALU.mult),
                    [pab, self.constb], [amb])
            pdbank = [[self.psbank() for hp in range(2)] for ee in range(2)]
            for ee in range(2):
                rows = slice(ee * 64, (ee + 1) * 64)
                for h in range(4):
                    pdt, pdb = pdbank[ee][h // 2]
                    self.mm(pdt[:, (h % 2) * 256:(h % 2 + 1) * 256], kendT[rows, h * 4 + blk, :],
                            vtok[rows, blk, h * 256:(h + 1) * 256], h % 2 == 0, h % 2 == 1,
                            [kendTb[h], vtokb[blk]], [pdb], h % 2 == 1, skip_group_check=True)
            pos = [self.psbank() for _ in range(2)]
            for h in range(4):
                pot, pob = pos[h // 2]
                base = (h % 2) * 256
                pd0, pd0b = pdbank[0][h // 2]
                pd1, pd1b = pdbank[1][h // 2]
                hc = slice((h % 2) * 256, (h % 2 + 1) * 256)
                n0 = blk * 2
                for dvc in range(2):
                    self.mm(pot[:, base + dvc * P:base + (dvc + 1) * P],
                            vtok[:, blk, h * 256 + dvc * P:h * 256 + (dvc + 1) * P], am[:, h * P:(h + 1) * P],
                            (h % 2 == 0 and dvc == 0), False, [vtokb[blk], amb], [pob], False,
                            skip_group_check=True)
                for dvc in range(2):
                    self.mm(pot[:, base + dvc * P:base + dvc * P + 64], SA[:, h, dvc * P:(dvc + 1) * P],
                            qd[:, h, blk * P:blk * P + 64], False, False, [SAb[h], qdb[h]], [pob], dvc == 1,
                            skip_group_check=True)
                self.op(self.dv, lambda e, h=h, pd0=pd0, n0=n0, hc=hc: e.scalar_tensor_tensor(
                    S[:, h, :], S[:, h, :], dec[:, h * 8 + n0:h * 8 + n0 + 1], pd0[:, hc], ALU.mult, ALU.add),
                    [Sb[h], decb, pd0b], [Sb[h]])
                self.act(SB[:, h, :], S[:, h, :], AF.Identity, [Sb[h]], [SBb[h]])
                for dvc in range(2):
                    self.mm(pot[:, base + dvc * P + 64:base + (dvc + 1) * P], SB[:, h, dvc * P:(dvc + 1) * P],
                            qd[:, h, blk * P + 64:(blk + 1) * P], False, True, [SBb[h], qdb[h]], [pob], dvc == 1,
                            skip_group_check=True)
                self.op(self.dv, lambda e, h=h, pd1=pd1, n0=n0, hc=hc: e.scalar_tensor_tensor(
                    S[:, h, :], S[:, h, :], dec[:, h * 8 + n0 + 1:h * 8 + n0 + 2], pd1[:, hc], ALU.mult, ALU.add),
                    [Sb[h], decb, pd1b], [Sb[h]])
                self.act(SA[:, h, :], S[:, h, :], AF.Identity, [Sb[h]], [SAb[h]])
            for i in range(2):
                pot, pob = pos[i]
                self.act(oT[:, 4 * i:4 * i + 4, tok], pot[:, :].rearrange("p (a b) -> p a b", b=P), AF.Identity,
                         [pob], oTb[4 * i:4 * i + 4])
                self.act(self.sq[:, 4 * i:4 * i + 4, tok], pot[:, :].rearrange("p (a b) -> p a b", b=P), AF.Square,
                         [pob], self.sqb[4 * i:4 * i + 4])

        if GLA_STAGE <= 3:
            return
        for h in range(4):
            pt, pb = self.psbank()
            for dvc in range(2):
                self.mm(pt[:, :], self.ones256[:, :], self.sq[:, 2 * h + dvc, :], dvc == 0, dvc == 1,
                        [self.sqb[2 * h + dvc], self.constb], [pb], dvc == 1)
            self.rstd_from(pt, pb)
            for dvc in range(2):
                c = 2 * h + dvc
                gcol = PRM_GN + j * 2 + dvc
                self.op(self.dv, lambda e, c=c, gcol=gcol: e.scalar_tensor_tensor(
                    oT[:, c, :], oT[:, c, :], self.prm[:, gcol:gcol + 1], self.rstd[:, :], ALU.mult, ALU.mult),
                    [oTb[c], self.rstdb, self.prmb], [oTb[c]])
                self.op(self.dv, lambda e, c=c: e.tensor_tensor(
                    mo[:, c, :], oT[:, c, :], self.yb_t[:, c, :], ALU.mult), [oTb[c], self.ybb[c]], [mob[c]])
        self.out_proj("gla_w_out", j, mo, mob, 1, l)


def _consts():
    c = np.zeros((P, CW), np.float32)
    c[:, C_ID:C_ID + P] = np.eye(P, dtype=np.float32)
    s = np.arange(P)[:, None]
    q = np.arange(P)[None, :]
    gm = ((s // 64 == q // 64) & (s <= q)).astype(np.float32)
    c[:, C_GMASK:C_GMASK + P] = gm
    c[:, C_GMASK4:C_GMASK4 + TT] = np.tile(gm, (1, 4))
    mcur = (s <= q)
    mprev = (s > q)
    c[:, C_MCUR:C_MCUR + P] = mcur.astype(np.float32)
    c[:, C_MPREV:C_MPREV + P] = mprev.astype(np.float32)
    c[:, C_NMCUR:C_NMCUR + TT] = np.tile(np.where(mcur, 0.0, -30000.0).astype(np.float32), (1, 4))
    c[:, C_NMPREV:C_NMPREV + TT] = np.tile(np.where(mprev, 0.0, -30000.0).astype(np.float32), (1, 4))
    tt = np.arange(TT)
    c[:, C_RMASK:C_RMASK + TT] = (tt % 64 != 0).astype(np.float32)[None, :]
    for cc in range(4):
        c[cc, C_IND + cc * P:C_IND + (cc + 1) * P] = 1.0
    return c


def _pack_params(inp):
    cols = []
    for k in ("ln_mix_pre", "ln_mix_post", "ln_mlp_pre", "ln_mlp_post"):
        cols.append(np.asarray(inp[k], np.float32).reshape(-1, P))
    cols.append(np.asarray(inp["gla_b_gate_up"], np.float32).reshape(-1, P))
    cols.append(np.asarray(inp["gla_g_norm"], np.float32).reshape(-1, P))
    bi = np.asarray(inp["swa_b_in"], np.float32)
    cols.append(bi.reshape(-1, P))
    cols.append(np.asarray(inp["swa_b_out"], np.float32).reshape(-1, P))
    for j in range(2):
        for kk in range(2):
            bk = bi[j, 1024 + kk * 64:1024 + (kk + 1) * 64]
            cols.append(np.concatenate([bk, bk])[None, :])
    prm = np.concatenate(cols, axis=0)
    assert prm.shape[0] == PRM_N, prm.shape
    return np.ascontiguousarray(prm.T)


def _pack_sinks(inp):
    s = np.asarray(inp["swa_sinks"], np.float32)
    out = np.zeros((4, 512), np.float32)
    for j in range(2):
        for kk in range(2):
            for par in range(2):
                col = ((j * 2 + kk) * 2 + par) * 64
                for cc in range(4):
                    out[cc, col:col + 64] = s[j, kk * 8 + 2 * cc + par]
    return out


FULL = [("gla", 0), ("mlp", 0), ("swa", 1), ("mlp", 1), ("gla", 2), ("mlp", 2), ("swa", 3), ("mlp", 3)]


def run(inp, sublayers=FULL, ntiles=SEQ // TT, ncores=8):
    x = np.asarray(inp["x"], np.float32)
    T = ntiles * TT
    kb = K(ntiles, sublayers)
    nc = kb.build()
    shared = {
        "prm": _pack_params(inp), "cst": _consts(), "snkL": _pack_sinks(inp),
        "bvrow": np.ascontiguousarray(np.asarray(inp["swa_b_in"], np.float32)[:, 1152:1280].reshape(1, 256)),
    }
    for k in ("gla_w_in", "gla_w_gate_up", "gla_w_out", "swa_w_in", "swa_w_out", "mlp_w_up", "mlp_w_down"):
        shared[k] = np.ascontiguousarray(np.asarray(inp[k], np.float32))
    in_maps = []
    for i in range(ncores):
        m = dict(shared)
        m["xT"] = np.ascontiguousarray(x[i, :T, :].T)
        in_maps.append(m)
    res = run_bass_kernel_spmd(nc, in_maps, core_ids=list(range(ncores)))
    out = np.stack([np.asarray(res.results[i]["yT"]).T for i in range(ncores)], axis=0)
    return np.ascontiguousarray(out.astype(np.float32))


def kernel(**inputs):
    return run(inputs)
```

```python
import numpy as np
import ml_dtypes
from contextlib import ExitStack

import concourse.bass as bass
import concourse.mybir as mybir
from concourse.bass_utils import run_bass_kernel_spmd

F32 = mybir.dt.float32
BF16 = mybir.dt.bfloat16
AF = mybir.ActivationFunctionType
ALU = mybir.AluOpType

P = 128
D = 1024
TT = 512
NCH = 8
DFF = 4096
SEQ = 4096
DEPTH = 4
EPS = 1e-6
GLA_IN = 3088
SWA_IN = 1280
SLAB_ELEMS = 8192
NSLAB = 3

PRM_LN = 0
PRM_GB = 128
PRM_GN = 136
PRM_SBI = 140
PRM_SBO = 160
PRM_KB = 176
PRM_N = 180

C_ID = 0
C_GMASK = 128
C_MCUR = 256
C_MPREV = 384
C_RMASK = 512
C_NMCUR = 1024
C_NMPREV = 1536
C_IND = 2048
C_GMASK4 = 2560
CW = 3072
ARENA = 18048
QSCALE = 128.0 ** -0.5
import os as _os
GLA_STAGE = int(_os.environ.get('GLA_STAGE', '9'))
GLA_SUB = _os.environ.get('GLA_SUB', 'z')


class Buf:
    __slots__ = ("name", "w", "r", "dsem", "dcnt")

    def __init__(self, name):
        self.name = name
        self.w = None
        self.r = {}
        self.dsem = None
        self.dcnt = 0


class Eng:
    def __init__(self, name, sem):
        self.name = name
        self.sem = sem
        self.cnt = 0
        self.seen = {}
        self.prog = []
        self.pending = None


class K:
    def __init__(self, ntiles, sublayers):
        self.ntiles = ntiles
        self.sublayers = sublayers
        self.nc = bass.Bass("TRN2", target_bir_lowering=False)
        self.es = ExitStack()
        self.nsem = 0
        self.pidx = 0

    def sem(self, name):
        self.nsem += 1
        return self.es.enter_context(self.nc.semaphore(name))

    def sb(self, name, shape, dt):
        return self.es.enter_context(self.nc.sbuf_tensor(name, list(shape), dt))

    def ps(self, name, shape, dt):
        return self.es.enter_context(self.nc.psum_tensor(name, list(shape), dt))

    def dram(self, name, shape, dt, kind):
        return self.nc.dram_tensor(name, list(shape), dt, kind=kind)

    def _waits(self, E, reads, writes):
        deps = {}

        def add(st):
            sem, val, eng = st
            if eng is E and E.name == "pe":
                return
            k = id(sem)
            if k not in deps or deps[k][1] < val:
                deps[k] = (sem, val)

        for b in reads:
            if b.w is not None:
                add(b.w)
        for b in writes:
            if b.w is not None and b.w[2] is not E:
                add(b.w)
            for st in b.r.values():
                if st[2] is not E:
                    add(st)
        for k, (sem, val) in deps.items():
            if E.seen.get(k, 0) < val:
                E.seen[k] = val
                E.prog.append(("wait", sem, val))

    def op(self, E, fn, reads=(), writes=(), inc=True):
        n0 = len(E.prog)
        self._waits(E, reads, writes)
        if len(E.prog) > n0 and E.pending is not None:
            i = E.pending
            E.prog[i] = ("op", E.prog[i][1], True)
            E.cnt += 1
            E.pending = None
        stamp = (E.sem, E.cnt + 1, E)
        E.prog.append(("op", fn, inc))
        if inc:
            E.cnt += 1
            E.pending = None
        else:
            E.pending = len(E.prog) - 1
        for b in reads:
            b.r[id(E.sem)] = stamp
        for b in writes:
            b.w = stamp
            b.r = {}

    def dma(self, Q, out_ap, in_ap, owner, reads=(), writes=()):
        self._waits(Q, reads, writes)
        if owner.dsem is None:
            owner.dsem = self.sem("d_" + owner.name)
        owner.dcnt += 16
        stamp = (owner.dsem, owner.dcnt, None)
        sem = owner.dsem
        Q.prog.append(("dma", out_ap, in_ap, sem))
        for b in reads:
            b.r[id(sem)] = stamp
        for b in writes:
            b.w = stamp
            b.r = {}

    def emit(self, E, eng):
        for it in E.prog:
            if it[0] == "wait":
                eng.wait_ge(it[1], it[2])
            elif it[0] == "op":
                ins = it[1](eng)
                if it[2]:
                    ins.then_inc(E.sem, 1)
            elif it[0] == "dma":
                eng.dma_start(out=it[1], in_=it[2]).then_inc(it[3], 16)

    def mm(self, out, lhsT, rhs, start, stop, reads, writes, inc, **kw):
        self.op(self.pe, lambda e: e.matmul(out, lhsT, rhs, start=start, stop=stop, **kw),
                reads, writes, inc)

    def act(self, out, in_, func, reads, writes, bias=None, scale=None):
        kw = {}
        if bias is not None:
            kw["bias"] = bias
        if scale is not None:
            kw["scale"] = scale
        self.op(self.ac, lambda e: e.activation(out=out, in_=in_, func=func, **kw), reads, writes)

    def barrier(self):
        engs = [self.pe, self.ac, self.dv, self.po]
        for E in engs[1:]:
            for Fe in engs:
                if Fe is E or Fe.cnt == 0:
                    continue
                k = id(Fe.sem)
                if E.seen.get(k, 0) < Fe.cnt:
                    E.seen[k] = Fe.cnt
                    E.prog.append(("wait", Fe.sem, Fe.cnt))
        self.aoff = 0

    def carve(self, words, dt=None, inner=None):
        a = self.arena[:, self.aoff:self.aoff + words]
        self.aoff += words
        assert self.aoff <= ARENA, self.aoff
        if dt is BF16:
            a = a.bitcast(BF16)
        if inner is not None:
            a = a.rearrange("p (a b) -> p a b", b=inner)
        return a

    def psbank(self):
        i = self.pidx % 8
        self.pidx += 1
        return self.pst[i], self.psb[i]

    def build(self):
        nc = self.nc
        nt = self.ntiles
        T = nt * TT
        self.pe = Eng("pe", self.sem("s_pe"))
        self.ac = Eng("act", self.sem("s_act"))
        self.dv = Eng("dve", self.sem("s_dve"))
        self.po = Eng("pool", self.sem("s_pool"))
        self.sp = Eng("sp", self.sem("s_sp"))

        dr = {}
        dr["xT"] = self.dram("xT", [D, T], F32, "ExternalInput")
        dr["yT"] = self.dram("yT", [D, T], F32, "ExternalOutput")
        dr["prm"] = self.dram("prm", [P, PRM_N], F32, "ExternalInput")
        dr["cst"] = self.dram("cst", [P, CW], F32, "ExternalInput")
        dr["snkL"] = self.dram("snkL", [4, 512], F32, "ExternalInput")
        dr["bvrow"] = self.dram("bvrow", [1, 256], F32, "ExternalInput")
        wshapes = {
            "gla_w_in": [2, D, GLA_IN], "gla_w_gate_up": [2, 16, 512], "gla_w_out": [2, D, D],
            "swa_w_in": [2, D, SWA_IN], "swa_w_out": [2, D, D],
            "mlp_w_up": [4, D, DFF], "mlp_w_down": [4, DFF, D],
        }
        wb = {}
        for k, shp in wshapes.items():
            dr[k] = self.dram(k, shp, F32, "ExternalInput")
            wb[k] = self.dram(k + "_bf", shp, BF16, "Internal")
        self.dr, self.wb = dr, wb

        self.xT = self.sb("xT_sb", [P, NCH, TT], F32)
        self.xTb = [Buf(f"xT{c}") for c in range(NCH)]
        self.hT = self.sb("hT_sb", [P, NCH, TT], BF16)
        self.hTb = [Buf(f"hT{c}") for c in range(NCH)]
        self.sq = self.sb("sq_sb", [P, NCH, TT], BF16)
        self.sqb = [Buf(f"sq{c}") for c in range(NCH)]
        self.yb_t = self.sb("y_sb", [P, NCH, TT], F32)
        self.ybb = [Buf(f"y{c}") for c in range(NCH)]
        self.rstd = self.sb("rstd_sb", [P, TT], F32)
        self.rstdb = Buf("rstd")
        self.slab = [self.sb(f"slab{i}", [P, SLAB_ELEMS], BF16) for i in range(NSLAB)]
        self.slabb = [Buf(f"slab{i}") for i in range(NSLAB)]
        self.slab_i = 0
        self.prm = self.sb("prm_sb", [P, PRM_N], F32)
        self.prmb = Buf("prm")
        self.arena = self.sb("arena_sb", [P, ARENA], F32)
        self.aoff = 0
        self.constb = Buf("const")
        self.ones = self.sb("ones_sb", [P, P], BF16)
        self.onesb = self.constb
        self.one1 = self.sb("one1_sb", [P, P], BF16)
        self.ones256 = self.sb("ones256_sb", [P, P], BF16)
        self.identb = self.sb("identb_sb", [P, P], BF16)
        self.identf = self.sb("identf_sb", [P, P], F32)
        self.gmask4 = self.sb("gmask4_sb", [P, TT], F32)
        self.rmask = self.sb("rmask_sb", [P, TT], F32)
        self.nmcur = self.sb("nmcur_sb", [P, TT], BF16)
        self.nmprev = self.sb("nmprev_sb", [P, TT], BF16)
        self.ind = self.sb("ind_sb", [4, TT], BF16)
        self.snkL = self.sb("snkL_sb", [4, 512], BF16)
        self.bvb = self.sb("bvb_sb", [1, 256], BF16)
        self.negb = self.sb("negb_sb", [P, 8], F32)
        self.eps = self.sb("eps_sb", [P, 1], F32)
        self.epsb = self.constb
        self.S = [self.sb(f"S{j}", [P, 4, 256], F32) for j in range(2)]
        self.Sb = [[Buf(f"S{j}_{h}") for h in range(4)] for j in range(2)]
        self.SA = [self.sb(f"SA{j}", [P, 4, 256], BF16) for j in range(2)]
        self.SAb = [[Buf(f"SA{j}_{h}") for h in range(4)] for j in range(2)]
        self.SB = self.sb("SB", [P, 4, 256], BF16)
        self.SBb = [Buf(f"SB_{h}") for h in range(4)]
        self.kd = [[self.sb(f"kd{j}_{kk}", [P, 640], BF16) for kk in range(2)] for j in range(2)]
        self.kdb = [[Buf(f"kd{j}_{kk}") for kk in range(2)] for j in range(2)]
        self.vts = [self.sb(f"vts{j}", [P, 5, P], BF16) for j in range(2)]
        self.vtsb = [Buf(f"vts{j}") for j in range(2)]
        self.wglr = [self.sb(f"wglr{j}", [P, NCH, 16], BF16) for j in range(2)]
        self.wglrb = [Buf(f"wglr{j}") for j in range(2)]
        self.wg = [self.sb(f"wg{j}", [16, 512], BF16) for j in range(2)]
        self.wgb = [Buf(f"wg{j}") for j in range(2)]
        self.p_i = 0
        self.pst = [self.ps(f"ps{i}", [P, TT], F32) for i in range(8)]
        self.psb = [Buf(f"ps{i}") for i in range(8)]

        self.dma(self.sp, self.prm[:, :], dr["prm"].ap(), self.prmb, writes=[self.prmb])
        cst = self.carve(CW)
        cstb = Buf("cst")
        self.dma(self.sp, cst, dr["cst"].ap(), cstb, writes=[cstb])
        snkst = self.carve(512)
        snkb = Buf("snkst")
        self.dma(self.sp, snkst[0:4, :], dr["snkL"].ap(), snkb, writes=[snkb])
        bvst = self.carve(256)
        bvstb = Buf("bvst")
        self.dma(self.sp, bvst[0:1, :], dr["bvrow"].ap(), bvstb, writes=[bvstb])
        po = self.po
        self.op(po, lambda e: e.memset(self.ones[:, :], 1.0 / D), writes=[self.constb])
        self.op(po, lambda e: e.memset(self.one1[:, :], 1.0), writes=[self.constb])
        self.op(po, lambda e: e.memset(self.ones256[:, :], 1.0 / 256.0), writes=[self.constb])
        self.op(po, lambda e: e.memset(self.eps[:, :], EPS), writes=[self.constb])
        for j in range(2):
            self.op(po, lambda e, j=j: e.memset(self.S[j][:, :, :], 0.0), writes=self.Sb[j])
            self.op(po, lambda e, j=j: e.memset(self.SA[j][:, :, :], 0.0), writes=self.SAb[j])
        self.op(po, lambda e: e.tensor_copy(self.identb[:, :], cst[:, C_ID:C_ID + P]), [cstb], [self.constb])
        self.op(po, lambda e: e.tensor_copy(self.identf[:, :], cst[:, C_ID:C_ID + P]), [cstb], [self.constb])
        self.op(po, lambda e: e.tensor_copy(self.gmask4[:, :], cst[:, C_GMASK4:C_GMASK4 + TT]), [cstb], [self.constb])
        self.op(po, lambda e: e.tensor_copy(self.rmask[:, :], cst[:, C_RMASK:C_RMASK + TT]), [cstb], [self.constb])
        self.op(po, lambda e: e.tensor_copy(self.nmcur[:, :], cst[:, C_NMCUR:C_NMCUR + TT]), [cstb], [self.constb])
        self.op(po, lambda e: e.tensor_copy(self.nmprev[:, :], cst[:, C_NMPREV:C_NMPREV + TT]), [cstb], [self.constb])
        self.op(po, lambda e: e.tensor_copy(self.ind[0:4, :], cst[0:4, C_IND:C_IND + TT]), [cstb], [self.constb])
        self.op(po, lambda e: e.tensor_copy(self.bvb[0:1, :], bvst[0:1, :]), [bvstb], [self.constb])
        self.op(po, lambda e: e.tensor_scalar(self.negb[:, :], self.prm[:, PRM_GB:PRM_GB + 8], -1.0, None, ALU.mult),
                [self.prmb], [self.constb])
        self.act(self.snkL[0:4, :], snkst[0:4, :], AF.Exp, [snkb], [self.constb])

        self.convb = {}

        def conv(k, j, c0, c1):
            key = (k, j, c0, c1)
            if key in self.convb:
                return
            b = Buf(f"cv_{k}_{j}_{c0}")
            self.convb[key] = b
            R = wshapes[k][1]
            rows_per = max(1, min(R, (1 << 19) // (c1 - c0)))
            r0 = 0
            while r0 < R:
                r1 = min(R, r0 + rows_per)
                self.dma(self.po, wb[k].ap()[j, r0:r1, c0:c1], dr[k].ap()[j, r0:r1, c0:c1], b, writes=[])
                r0 = r1
            b.w = (b.dsem, b.dcnt, None)

        for (kind, l) in self.sublayers:
            j = l // 2
            if kind == "gla":
                conv("gla_w_in", j, 3072, 3088)
                conv("gla_w_gate_up", j, 0, 512)
                conv("gla_w_in", j, 0, 1024)
                conv("gla_w_in", j, 1024, 2048)
                conv("gla_w_in", j, 2048, 3072)
                conv("gla_w_out", j, 0, 1024)
            elif kind == "swa":
                conv("swa_w_in", j, 0, 1024)
                conv("swa_w_in", j, 1024, 1280)
                conv("swa_w_out", j, 0, 1024)
            elif kind == "mlp":
                for s_ in range(4):
                    conv("mlp_w_up", l, s_ * 1024, (s_ + 1) * 1024)
                for g in range(4):
                    conv("mlp_w_down", l, g * 256, (g + 1) * 256)
        self.small_loaded = set()
        self.barrier()

        xTd = dr["xT"].ap().rearrange("(c p) t -> p c t", p=P)
        yTd = dr["yT"].ap().rearrange("(c p) t -> p c t", p=P)
        self.xin = [Buf(f"xin{c}") for c in range(NCH)]
        self.xout = [Buf(f"xout{c}") for c in range(NCH)]
        for t in range(nt):
            tsl = slice(t * TT, (t + 1) * TT)
            for c in range(NCH):
                self.dma(self.sp, self.xT[:, c, :], xTd[:, c, tsl], self.xin[c], writes=[self.xTb[c]])
            for (kind, l) in self.sublayers:
                if kind == "mlp":
                    self.mlp(l)
                elif kind == "gla":
                    self.gla(l, t)
                elif kind == "swa":
                    self.swa(l, t)
            for c in range(NCH):
                self.dma(self.sp, yTd[:, c, tsl], self.xT[:, c, :], self.xout[c], reads=[self.xTb[c]])
        for c in range(NCH):
            self.sp.prog.append(("wait", self.xout[c].dsem, self.xout[c].dcnt))

        with nc.allow_low_precision("bf16 matmul operands, fp32 accumulation"):
            with nc.Block() as block:
                @block.tensor
                def _(e):
                    self.emit(self.pe, e)

                @block.scalar
                def _(e):
                    self.emit(self.ac, e)

                @block.vector
                def _(e):
                    self.emit(self.dv, e)

                @block.gpsimd
                def _(e):
                    self.emit(self.po, e)

                @block.sync
                def _(e):
                    self.emit(self.sp, e)
        self.es.close()
        return nc

    def gain_ap(self, kind, l, c):
        col = PRM_LN + kind * 32 + l * 8 + c
        return self.prm[:, col:col + 1]

    def slab_load(self, key, j, cols):
        i = self.slab_i % NSLAB
        self.slab_i += 1
        st, sbuf = self.slab[i], self.slabb[i]
        w = self.wb[key].ap()[j]
        c0, c1 = cols
        n = c1 - c0
        src = w.rearrange("(a p) f -> p a f", p=P)[:, :, c0:c1]
        na = src.shape[1]
        dst = st[:, 0:na * n].rearrange("p (a f) -> p a f", f=n)
        self.dma(self.sp, dst, src, sbuf, reads=[self.convb[(key, j, c0, c1)]], writes=[sbuf])
        return dst, sbuf

    def load_small(self, j):
        if j in self.small_loaded:
            return
        self.small_loaded.add(j)
        src = self.wb["gla_w_in"].ap()[j].rearrange("(a p) f -> p a f", p=P)[:, :, 3072:3088]
        self.dma(self.sp, self.wglr[j][:, :, :], src, self.wglrb[j],
                 reads=[self.convb[("gla_w_in", j, 3072, 3088)]], writes=[self.wglrb[j]])
        self.dma(self.sp, self.wg[j][:, :], self.wb["gla_w_gate_up"].ap()[j], self.wgb[j],
                 reads=[self.convb[("gla_w_gate_up", j, 0, 512)]], writes=[self.wgb[j]])

    def sumsq_rstd(self, srcb):
        pt, pb = self.psbank()
        for c in range(NCH):
            self.mm(pt[:, :], self.ones[:, :], self.sq[:, c, :], c == 0, c == NCH - 1,
                    [self.sqb[c], self.onesb], [pb], c == NCH - 1)
        self.rstd_from(pt, pb)

    def rstd_from(self, pt, pb):
        self.act(self.rstd[:, :], pt[:, :], AF.Ln, [pb, self.constb], [self.rstdb], bias=self.eps[:, 0:1])
        self.act(self.rstd[:, :], self.rstd[:, :], AF.Exp, [self.rstdb], [self.rstdb], scale=-0.5)

    def prenorm(self, kind, l):
        for c in range(NCH):
            self.act(self.sq[:, c, :], self.xT[:, c, :], AF.Square, [self.xTb[c]], [self.sqb[c]])
        self.sumsq_rstd(None)
        for c in range(NCH):
            g = self.gain_ap(kind, l, c)
            self.op(self.dv, lambda e, c=c, g=g: e.scalar_tensor_tensor(
                self.hT[:, c, :], self.xT[:, c, :], g, self.rstd[:, :], ALU.mult, ALU.mult),
                [self.xTb[c], self.rstdb, self.prmb], [self.hTb[c]])

    def postnorm_residual(self, kind, l):
        self.sumsq_rstd(None)
        for c in range(NCH):
            g = self.gain_ap(kind, l, c)
            self.op(self.dv, lambda e, c=c, g=g: e.scalar_tensor_tensor(
                self.yb_t[:, c, :], self.yb_t[:, c, :], g, self.rstd[:, :], ALU.mult, ALU.mult),
                [self.ybb[c], self.rstdb, self.prmb], [self.ybb[c]])
            self.op(self.dv, lambda e, c=c: e.tensor_tensor(
                self.xT[:, c, :], self.xT[:, c, :], self.yb_t[:, c, :], ALU.add),
                [self.ybb[c], self.xTb[c]], [self.xTb[c]])

    def evac_y(self, pt, pb, c, bias=None):
        self.act(self.yb_t[:, c, :], pt[:, :], AF.Identity, [pb] + ([self.prmb] if bias is not None else []),
                 [self.ybb[c]], bias=bias)
        self.act(self.sq[:, c, :], pt[:, :], AF.Square, [pb] + ([self.prmb] if bias is not None else []),
                 [self.sqb[c]], bias=bias)

    def mlp(self, l):
        self.prenorm(2, l)
        self.barrier()
        hid = self.carve(8192, BF16, TT)
        hidb = [Buf(f"hid{c}") for c in range(32)]
        relu = [self.carve(512) for _ in range(3)]
        relub = [Buf(f"relu{i}") for i in range(3)]
        for s_ in range(4):
            w, wbuf = self.slab_load("mlp_w_up", l, (s_ * 1024, (s_ + 1) * 1024))
            for j in range(8):
                fch = s_ * 8 + j
                pt, pb = self.psbank()
                for kc in range(NCH):
                    self.mm(pt[:, :], w[:, kc, j * P:(j + 1) * P], self.hT[:, kc, :],
                            kc == 0, kc == NCH - 1, [wbuf, self.hTb[kc]], [pb], kc == NCH - 1)
                ri = fch % 3
                rt, rb = relu[ri], relub[ri]
                self.act(rt[:, :], pt[:, :], AF.Relu, [pb], [rb])
                self.op(self.dv, lambda e, fch=fch, rt=rt: e.tensor_tensor(
                    hid[:, fch, :], rt[:, :], rt[:, :], ALU.mult),
                    [rb], [hidb[fch]])
        for g in range(4):
            w, wbuf = self.slab_load("mlp_w_down", l, (g * 256, (g + 1) * 256))
            for c2 in range(2):
                c = 2 * g + c2
                pt, pb = self.psbank()
                for fc in range(32):
                    self.mm(pt[:, :], w[:, fc, c2 * P:(c2 + 1) * P], hid[:, fc, :],
                            fc == 0, fc == 31, [wbuf, hidb[fc]], [pb], fc == 31)
                self.evac_y(pt, pb, c)
        self.postnorm_residual(3, l)

    def proj_fm(self, w, wbuf, col0, nchunks, sink):
        for i in range(nchunks):
            pt, pb = self.psbank()
            for kc in range(NCH):
                self.mm(pt[:, :], w[:, kc, col0 + i * P:col0 + (i + 1) * P], self.hT[:, kc, :],
                        kc == 0, kc == NCH - 1, [wbuf, self.hTb[kc]], [pb], kc == NCH - 1)
            sink(i, pt, pb)

    def out_proj(self, key, j, mo, mob, kind, l, bias_col0=None):
        w, wbuf = self.slab_load(key, j, (0, 1024))
        for dc in range(8):
            pt, pb = self.psbank()
            for c in range(8):
                self.mm(pt[:, :], w[:, c, dc * P:(dc + 1) * P], mo[:, c, :], c == 0, c == 7,
                        [wbuf, mob[c]], [pb], c == 7)
            bias = None
            if bias_col0 is not None:
                bias = self.prm[:, bias_col0 + dc:bias_col0 + dc + 1]
            self.evac_y(pt, pb, dc, bias=bias)
        self.postnorm_residual(kind, l)

    def swa(self, l, t):
        j = l // 2
        self.prenorm(0, l)
        self.barrier()
        qT = self.carve(2048, BF16, TT)
        qTb = [Buf(f"qT{c}") for c in range(8)]
        mo = self.carve(2048, BF16, TT)
        mob = [Buf(f"mo{c}") for c in range(8)]
        pT = [self.carve(256, BF16) for _ in range(8)]
        pTb = [Buf(f"pT{i}") for i in range(8)]
        rd = [self.carve(512) for _ in range(2)]
        rdb = [Buf(f"rd{i}") for i in range(2)]
        kd, kdb = self.kd[j], self.kdb[j]
        vt, vtb = self.vts[j], self.vtsb[j]

        w, wbuf = self.slab_load("swa_w_in", j, (0, 1024))

        def qsink(c, pt, pb):
            col = PRM_SBI + j * 10 + c
            self.act(qT[:, c, :], pt[:, :], AF.Identity, [pb, self.prmb], [qTb[c]],
                     bias=self.prm[:, col:col + 1])
        self.proj_fm(w, wbuf, 0, 8, qsink)

        w, wbuf = self.slab_load("swa_w_in", j, (1024, 1280))
        for kk in range(2):
            pt, pb = self.psbank()
            for half in range(2):
                for kc in range(NCH):
                    self.mm(pt[half * 64:(half + 1) * 64, :], w[:, kc, kk * 64:(kk + 1) * 64],
                            self.hT[:, kc, :], kc == 0, kc == NCH - 1, [wbuf, self.hTb[kc]], [pb],
                            kc == NCH - 1 and half == 1)
            col = PRM_KB + j * 2 + kk
            self.act(kd[kk][:, 128:640], pt[:, :], AF.Identity, [pb, self.prmb], [kdb[kk]],
                     bias=self.prm[:, col:col + 1])
        pt, pb = self.psbank()
        for blk in range(4):
            for kc in range(NCH):
                self.mm(pt[:, blk * P:(blk + 1) * P], self.hT[:, kc, blk * P:(blk + 1) * P],
                        w[:, kc, 128:256], kc == 0, False, [wbuf, self.hTb[kc]], [pb], False)
            self.mm(pt[:, blk * P:(blk + 1) * P], self.one1[0:1, :], self.bvb[0:1, j * P:(j + 1) * P],
                    False, True, [self.constb], [pb], blk == 3)
        self.act(vt[:, 1:5, :], pt[:, :].rearrange("p (a b) -> p a b", b=P), AF.Identity, [pb], [vtb])

        for blk in range(4):
            gblk = t * 4 + blk
            tok = slice(blk * P, (blk + 1) * P)
            for kk in range(2):
                qbufs = qTb[kk * 4:(kk + 1) * 4]
                kbs = ([] if gblk == 0 else [0]) + [1]
                ptiles = {}
                for kb in kbs:
                    kcol = blk * P + kb * P
                    nm = self.nmprev if kb == 0 else self.nmcur
                    for par in range(2):
                        pt, pb = self.psbank()
                        rows = slice(par * 64, (par + 1) * 64)
                        self.mm(pt[:, :], kd[kk][rows, kcol:kcol + P], qT[rows, kk * 4:(kk + 1) * 4, tok],
                                True, False, [kdb[kk]] + qbufs, [pb], False)
                        self.mm(pt[:, :], self.identb[:, :], nm[:, :], False, True, [self.constb], [pb], True)
                        pi = self.p_i % 8
                        self.p_i += 1
                        self.act(pT[pi][:, :], pt[:, :], AF.Exp, [pb], [pTb[pi]], scale=0.125)
                        ptiles[(kb, par)] = (pT[pi], pTb[pi])
                po_t, po_b = self.psbank()
                pd_t, pd_b = self.psbank()
                for par in range(2):
                    rows = slice(par * 64, (par + 1) * 64)
                    for i, kb in enumerate(kbs):
                        ptile, pbuf = ptiles[(kb, par)]
                        self.mm(po_t[rows, :], vt[:, blk + kb, kk * 64:(kk + 1) * 64], ptile[:, :],
                                i == 0, i == len(kbs) - 1, [vtb, pbuf], [po_b], False)
                    for i, kb in enumerate(kbs):
                        ptile, pbuf = ptiles[(kb, par)]
                        self.mm(pd_t[rows, :], self.one1[:, 0:64], ptile[:, :], i == 0, False,
                                [pbuf, self.constb], [pd_b], False)
                    scol = ((j * 2 + kk) * 2 + par) * 64
                    self.mm(pd_t[rows, :], self.snkL[0:4, scol:scol + 64], self.ind[0:4, :], False, True,
                            [self.constb], [pd_b], par == 1)
                ri = (blk * 2 + kk) % 2
                self.act(rd[ri][:, :], pd_t[:, :], AF.Ln, [pd_b], [rdb[ri]])
                self.act(rd[ri][:, :], rd[ri][:, :], AF.Exp, [rdb[ri]], [rdb[ri]], scale=-1.0)
                self.op(self.dv, lambda e, ri=ri, po_t=po_t, kk=kk, tok=tok: e.tensor_tensor(
                    mo[:, kk * 4:(kk + 1) * 4, tok], po_t[:, :].rearrange("p (a b) -> p a b", b=P),
                    rd[ri][:, :].rearrange("p (a b) -> p a b", b=P), ALU.mult),
                    [po_b, rdb[ri]], mob[kk * 4:(kk + 1) * 4])
        for kk in range(2):
            self.op(self.dv, lambda e, kk=kk: e.tensor_copy(kd[kk][:, 0:128], kd[kk][:, 512:640]),
                    [kdb[kk]], [kdb[kk]])
        self.op(self.dv, lambda e: e.tensor_copy(vt[:, 0, :], vt[:, 4, :]), [vtb], [vtb])
        self.out_proj("swa_w_out", j, mo, mob, 1, l, bias_col0=PRM_SBO + j * 8)

    def gla(self, l, t):
        j = l // 2
        self.load_small(j)
        self.prenorm(0, l)
        self.barrier()
        qd = self.carve(1024, BF16, TT)
        qdb = [Buf(f"qd{h}") for h in range(4)]
        kinv = self.carve(1024, BF16, TT)
        kinvb = [Buf(f"kinv{h}") for h in range(4)]
        kend = self.carve(2048, None, TT)
        kendb = [Buf(f"kend{h}") for h in range(4)]
        kendT = self.carve(1024, BF16, P)
        kendTb = [Buf(f"kendT{h}") for h in range(4)]
        vtok = self.carve(2048, BF16, 1024)
        vtokb = [Buf(f"vtok{b}") for b in range(4)]
        la = self.carve(512)
        lab = Buf("la")
        cpad = [self.carve(768, None, 96) for _ in range(2)]
        cpadb = [Buf(f"cpad{i}") for i in range(2)]
        for i in range(2):
            self.op(self.dv, lambda e, i=i: e.memset(cpad[i][:, :, 0:32], 0.0), [], [cpadb[i]])
        E1 = [self.carve(512) for _ in range(2)]
        E1b = [Buf(f"E1_{i}") for i in range(2)]
        E2 = [self.carve(512) for _ in range(2)]
        E2b = [Buf(f"E2_{i}") for i in range(2)]
        oT = self.carve(4096, None, TT)
        oTb = [Buf(f"oT{c}") for c in range(8)]
        mo = self.carve(2048, BF16, TT)
        mob = [Buf(f"mo{c}") for c in range(8)]
        am = self.carve(256, BF16)
        amb = Buf("am")
        glrT = self.carve(256, BF16)
        glrTb = Buf("glrT")
        dec = self.carve(32)
        decb = Buf("dec")
        S, Sb = self.S[j], self.Sb[j]
        SA, SAb = self.SA[j], self.SAb[j]
        SB, SBb = self.SB, self.SBb

        wA, wAb = self.slab_load("gla_w_in", j, (0, 1024))
        wB, wBb = self.slab_load("gla_w_in", j, (1024, 2048))
        wC, wCb = self.slab_load("gla_w_in", j, (2048, 3072))
        pt, pb = self.psbank()
        for kc in range(NCH):
            self.mm(pt[0:16, :], self.wglr[j][:, kc, :], self.hT[:, kc, :], kc == 0, kc == NCH - 1,
                    [self.wglrb[j], self.hTb[kc]], [pb], kc == NCH - 1)
        self.act(glrT[0:16, :], pt[0:16, :], AF.Identity, [pb], [glrTb])

        def chain(h):
            pz, pzb = self.psbank()
            self.mm(pz[:, :], self.wg[j][0:16, h * P:(h + 1) * P], glrT[0:16, :], True, True,
                    [self.wgb[j], glrTb], [pzb], True)
            col = j * 4 + h
            self.act(la[:, :], pz[:, :], AF.Exp, [pzb, self.constb], [lab],
                     bias=self.negb[:, col:col + 1], scale=-1.0)
            self.act(la[:, :], la[:, :], AF.Ln, [lab], [lab], bias=1.0)
            src, srcb = cpad[0], cpadb[0]
            self.op(self.dv, lambda e, src=src: e.tensor_copy(src[:, :, 32:96], la[:, :].rearrange("p (a b) -> p a b", b=64)),
                    [lab], [srcb])
            k = 0
            for d in (1, 2, 4, 8, 16, 32):
                dst, dstb = cpad[1 - k], cpadb[1 - k]
                self.op(self.dv, lambda e, src=src, dst=dst, d=d: e.tensor_tensor(
                    dst[:, :, 32:96], src[:, :, 32:96], src[:, :, 32 - d:96 - d], ALU.add), [srcb], [dstb])
                src, srcb = dst, dstb
                k = 1 - k
            cumv = src[:, :, 32:96]
            e1, e1b = E1[h % 2], E1b[h % 2]
            e2, e2b = E2[h % 2], E2b[h % 2]
            self.act(e1[:, :].rearrange("p (a b) -> p a b", b=64), cumv, AF.Exp, [srcb], [e1b], scale=-1.0 / 16.0)
            self.act(e2[:, :].rearrange("p (a b) -> p a b", b=64), cumv, AF.Exp, [srcb], [e2b], scale=1.0 / 16.0)
            self.act(dec[:, h * 8:(h + 1) * 8], src[:, :, 95], AF.Exp, [srcb], [decb], scale=-1.0 / 16.0)

        def qk(h):
            e1, e1b = E1[h % 2], E1b[h % 2]
            e2, e2b = E2[h % 2], E2b[h % 2]
            pq, pqb = self.psbank()
            for kc in range(NCH):
                self.mm(pq[:, :], wA[:, kc, h * P:(h + 1) * P], self.hT[:, kc, :], kc == 0, kc == NCH - 1,
                        [wAb, self.hTb[kc]], [pqb], kc == NCH - 1)
            pk, pkb = self.psbank()
            for kc in range(NCH):
                self.mm(pk[:, :], wA[:, kc, 512 + h * P:512 + (h + 1) * P], self.hT[:, kc, :], kc == 0,
                        kc == NCH - 1, [wAb, self.hTb[kc]], [pkb], kc == NCH - 1)
            self.op(self.dv, lambda e, h=h, pq=pq, e1=e1: e.scalar_tensor_tensor(
                qd[:, h, :], pq[:, :], QSCALE, e1[:, :], ALU.mult, ALU.mult), [pqb, e1b], [qdb[h]])
            self.op(self.dv, lambda e, h=h, pk=pk, e2=e2: e.tensor_tensor(
                kinv[:, h, :], pk[:, :], e2[:, :], ALU.mult), [pkb, e2b], [kinvb[h]])
            for n in range(8):
                cs = slice(n * 64, (n + 1) * 64)
                self.op(self.dv, lambda e, h=h, pk=pk, e2=e2, n=n, cs=cs: e.scalar_tensor_tensor(
                    kend[:, h, cs], pk[:, cs], dec[:, h * 8 + n:h * 8 + n + 1], e2[:, cs], ALU.mult, ALU.mult),
                    [pkb, e2b, decb], [kendb[h]])

        def tr(h):
            ptr, ptrb = self.psbank()
            for blk in range(4):
                self.op(self.pe, lambda e, h=h, blk=blk, ptr=ptr: e.transpose(
                    ptr[:, blk * P:(blk + 1) * P], kend[:, h, blk * P:(blk + 1) * P], self.identf[:, :]),
                    [kendb[h], self.constb], [ptrb], blk == 3)
            self.act(kendT[:, h * 4:(h + 1) * 4, :], ptr[:, :].rearrange("p (a b) -> p a b", b=P),
                     AF.Identity, [ptrb], [kendTb[h]])

        def vproj(blk):
            for half in range(2):
                pt, pb = self.psbank()
                for kc in range(NCH):
                    self.mm(pt[:, :], self.hT[:, kc, blk * P:(blk + 1) * P], wB[:, kc, half * 512:(half + 1) * 512],
                            kc == 0, kc == NCH - 1, [wBb, self.hTb[kc]], [pb], kc == NCH - 1)
                self.act(vtok[:, blk, half * 512:(half + 1) * 512], pt[:, :], AF.Identity, [pb], [vtokb[blk]])

        def gproj(c):
            pt, pb = self.psbank()
            for kc in range(NCH):
                self.mm(pt[:, :], wC[:, kc, c * P:(c + 1) * P], self.hT[:, kc, :],
                        kc == 0, kc == NCH - 1, [wCb, self.hTb[kc]], [pb], kc == NCH - 1)
            self.act(self.yb_t[:, c, :], pt[:, :], AF.Silu, [pb], [self.ybb[c]])

        chain(0)
        for h in range(4):
            vproj(h)
            if h + 1 < 4:
                chain(h + 1)
            gproj(2 * h)
            gproj(2 * h + 1)
            qk(h)
            if h >= 1:
                tr(h - 1)
        tr(3)

        if GLA_STAGE <= 2:
            return
        for blk in range(4):
            tok = slice(blk * P, (blk + 1) * P)
            pa, pab = self.psbank()
            for h in range(4):
                self.mm(pa[:, h * P:(h + 1) * P], kinv[:, h, tok], qd[:, h, tok], h == 0, h == 3,
                        [kinvb[h], qdb[h]], [pab], h == 3, skip_group_check=True)
            self.op(self.dv, lambda e, pa=pa: e.tensor_tensor(am[:, :], pa[:, :], self.gmask4[:, :], ALU.mult),
                    [pab, self.constb], [amb])
            pdbank = [[self.psbank() for hp in range(2)] for ee in range(2)]
            for ee in range(2):
                rows = slice(ee * 64, (ee + 1) * 64)
                for h in range(4):
                    pdt, pdb = pdbank[ee][h // 2]
                    self.mm(pdt[:, (h % 2) * 256:(h % 2 + 1) * 256], kendT[rows, h * 4 + blk, :],
                            vtok[rows, blk, h * 256:(h + 1) * 256], h % 2 == 0, h % 2 == 1,
                            [kendTb[h], vtokb[blk]], [pdb], h % 2 == 1, skip_group_check=True)
            pos = [self.psbank() for _ in range(2)]
            n0 = blk * 2
            for h in range(4):
                pot, pob = pos[h // 2]
                base = (h % 2) * 256
                for dvc in range(2):
                    self.mm(pot[:, base + dvc * P:base + (dvc + 1) * P],
                            vtok[:, blk, h * 256 + dvc * P:h * 256 + (dvc + 1) * P], am[:, h * P:(h + 1) * P],
                            (h % 2 == 0 and dvc == 0), False, [vtokb[blk], amb], [pob], False,
                            skip_group_check=True)
                for dvc in range(2):
                    self.mm(pot[:, base + dvc * P:base + dvc * P + 64], SA[:, h, dvc * P:(dvc + 1) * P],
                            qd[:, h, blk * P:blk * P + 64], False, False, [SAb[h], qdb[h]], [pob], dvc == 1,
                            skip_group_check=True)
            for h in range(4):
                pd0, pd0b = pdbank[0][h // 2]
                hc = slice((h % 2) * 256, (h % 2 + 1) * 256)
                self.op(self.dv, lambda e, h=h, pd0=pd0, hc=hc, n0=n0: e.scalar_tensor_tensor(
                    S[:, h, :], S[:, h, :], dec[:, h * 8 + n0:h * 8 + n0 + 1], pd0[:, hc], ALU.mult, ALU.add),
                    [Sb[h], decb, pd0b], [Sb[h]])
                self.act(SB[:, h, :], S[:, h, :], AF.Identity, [Sb[h]], [SBb[h]])
            for h in range(4):
                pot, pob = pos[h // 2]
                base = (h % 2) * 256
                for dvc in range(2):
                    self.mm(pot[:, base + dvc * P + 64:base + (dvc + 1) * P], SB[:, h, dvc * P:(dvc + 1) * P],
                            qd[:, h, blk * P + 64:(blk + 1) * P], False, True, [SBb[h], qdb[h]], [pob], dvc == 1,
                            skip_group_check=True)
            for h in range(4):
                pd1, pd1b = pdbank[1][h // 2]
                hc = slice((h % 2) * 256, (h % 2 + 1) * 256)
                self.op(self.dv, lambda e, h=h, pd1=pd1, hc=hc, n0=n0: e.scalar_tensor_tensor(
                    S[:, h, :], S[:, h, :], dec[:, h * 8 + n0 + 1:h * 8 + n0 + 2], pd1[:, hc], ALU.mult, ALU.add),
                    [Sb[h], decb, pd1b], [Sb[h]])
                self.act(SA[:, h, :], S[:, h, :], AF.Identity, [Sb[h]], [SAb[h]])
            for i in range(2):
                pot, pob = pos[i]
                self.act(oT[:, 4 * i:4 * i + 4, tok], pot[:, :].rearrange("p (a b) -> p a b", b=P), AF.Identity,
                         [pob], oTb[4 * i:4 * i + 4])
                self.act(self.sq[:, 4 * i:4 * i + 4, tok], pot[:, :].rearrange("p (a b) -> p a b", b=P), AF.Square,
                         [pob], self.sqb[4 * i:4 * i + 4])

        if GLA_STAGE <= 3:
            return
        for h in range(4):
            pt, pb = self.psbank()
            for dvc in range(2):
                self.mm(pt[:, :], self.ones256[:, :], self.sq[:, 2 * h + dvc, :], dvc == 0, dvc == 1,
                        [self.sqb[2 * h + dvc], self.constb], [pb], dvc == 1)
            self.rstd_from(pt, pb)
            for dvc in range(2):
                c = 2 * h + dvc
                gcol = PRM_GN + j * 2 + dvc
                self.op(self.dv, lambda e, c=c, gcol=gcol: e.scalar_tensor_tensor(
                    oT[:, c, :], oT[:, c, :], self.prm[:, gcol:gcol + 1], self.rstd[:, :], ALU.mult, ALU.mult),
                    [oTb[c], self.rstdb, self.prmb], [oTb[c]])
                self.op(self.dv, lambda e, c=c: e.tensor_tensor(
                    mo[:, c, :], oT[:, c, :], self.yb_t[:, c, :], ALU.mult), [oTb[c], self.ybb[c]], [mob[c]])
        self.out_proj("gla_w_out", j, mo, mob, 1, l)


def _consts():
    c = np.zeros((P, CW), np.float32)
    c[:, C_ID:C_ID + P] = np.eye(P, dtype=np.float32)
    s = np.arange(P)[:, None]
    q = np.arange(P)[None, :]
    gm = ((s // 64 == q // 64) & (s <= q)).astype(np.float32)
    c[:, C_GMASK:C_GMASK + P] = gm
    c[:, C_GMASK4:C_GMASK4 + TT] = np.tile(gm, (1, 4))
    mcur = (s <= q)
    mprev = (s > q)
    c[:, C_MCUR:C_MCUR + P] = mcur.astype(np.float32)
    c[:, C_MPREV:C_MPREV + P] = mprev.astype(np.float32)
    c[:, C_NMCUR:C_NMCUR + TT] = np.tile(np.where(mcur, 0.0, -30000.0).astype(np.float32), (1, 4))
    c[:, C_NMPREV:C_NMPREV + TT] = np.tile(np.where(mprev, 0.0, -30000.0).astype(np.float32), (1, 4))
    tt = np.arange(TT)
    c[:, C_RMASK:C_RMASK + TT] = (tt % 64 != 0).astype(np.float32)[None, :]
    for cc in range(4):
        c[cc, C_IND + cc * P:C_IND + (cc + 1) * P] = 1.0
    return c


def _pack_params(inp):
    cols = []
    for k in ("ln_mix_pre", "ln_mix_post", "ln_mlp_pre", "ln_mlp_post"):
        cols.append(np.asarray(inp[k], np.float32).reshape(-1, P))
    cols.append(np.asarray(inp["gla_b_gate_up"], np.float32).reshape(-1, P))
    cols.append(np.asarray(inp["gla_g_norm"], np.float32).reshape(-1, P))
    bi = np.asarray(inp["swa_b_in"], np.float32)
    cols.append(bi.reshape(-1, P))
    cols.append(np.asarray(inp["swa_b_out"], np.float32).reshape(-1, P))
    for j in range(2):
        for kk in range(2):
            bk = bi[j, 1024 + kk * 64:1024 + (kk + 1) * 64]
            cols.append(np.concatenate([bk, bk])[None, :])
    prm = np.concatenate(cols, axis=0)
    assert prm.shape[0] == PRM_N, prm.shape
    return np.ascontiguousarray(prm.T)


def _pack_sinks(inp):
    s = np.asarray(inp["swa_sinks"], np.float32)
    out = np.zeros((4, 512), np.float32)
    for j in range(2):
        for kk in range(2):
            for par in range(2):
                col = ((j * 2 + kk) * 2 + par) * 64
                for cc in range(4):
                    out[cc, col:col + 64] = s[j, kk * 8 + 2 * cc + par]
    return out


FULL = [("gla", 0), ("mlp", 0), ("swa", 1), ("mlp", 1), ("gla", 2), ("mlp", 2), ("swa", 3), ("mlp", 3)]


def run(inp, sublayers=FULL, ntiles=SEQ // TT, ncores=8):
    x = np.asarray(inp["x"], np.float32)
    T = ntiles * TT
    kb = K(ntiles, sublayers)
    nc = kb.build()
    shared = {
        "prm": _pack_params(inp), "cst": _consts(), "snkL": _pack_sinks(inp),
        "bvrow": np.ascontiguousarray(np.asarray(inp["swa_b_in"], np.float32)[:, 1152:1280].reshape(1, 256)),
    }
    for k in ("gla_w_in", "gla_w_gate_up", "gla_w_out", "swa_w_in", "swa_w_out", "mlp_w_up", "mlp_w_down"):
        shared[k] = np.ascontiguousarray(np.asarray(inp[k], np.float32))
    in_maps = []
    for i in range(ncores):
        m = dict(shared)
        m["xT"] = np.ascontiguousarray(x[i, :T, :].T)
        in_maps.append(m)
    res = run_bass_kernel_spmd(nc, in_maps, core_ids=list(range(ncores)))
    out = np.stack([np.asarray(res.results[i]["yT"]).T for i in range(ncores)], axis=0)
    return np.ascontiguousarray(out.astype(np.float32))


def kernel(**inputs):
    return run(inputs)
```
